# Optimizing a Trainium2 kernel written in Bass

```python
import math
import jax
import jax.numpy as jnp
from jax import lax
import numpy as np

D_MODEL = 1024
BATCH = 4
SEQ = 4096
DEPTH = 1

HG_HEADS = 4
HG_DK = 128
HG_DV = 128
HG_WIDTH = HG_HEADS * HG_DK
HG_CHUNK = 64
ATT_BRANCHES = ((128, 1), (512, 4), (2048, 16))
ATT_HEADS_PER_BRANCH = 4
ATT_HEAD_DIM = 64
ATT_HEADS = ATT_HEADS_PER_BRANCH * len(ATT_BRANCHES)
ATT_WIDTH = ATT_HEADS * ATT_HEAD_DIM
ATT_OUT_WIDTH = ATT_HEADS_PER_BRANCH * ATT_HEAD_DIM
REL_BUCKETS = 32
REL_MAX_DIST = 2048
N_EXPERTS = 256
TOP_K = 8
N_GROUPS = 8
TOPK_GROUPS = 4
EXPERT_DIM = 256
SHARED_DIM = 256
ROUTED_SCALE = 2.5
MOE_BLOCK = 128
DN_ALPHA = (2 * DEPTH) ** 0.25
DN_BETA = (8 * DEPTH) ** -0.25
LN_EPS = 1e-5
RMS_EPS = 1e-6
IN_WIDTH = 4 * HG_WIDTH + 3 * ATT_WIDTH
IN_SPLITS = (HG_WIDTH, 2 * HG_WIDTH, 3 * HG_WIDTH, 4 * HG_WIDTH,
             4 * HG_WIDTH + ATT_WIDTH, 4 * HG_WIDTH + 2 * ATT_WIDTH)
MIX_OUT = HG_WIDTH + ATT_OUT_WIDTH

kernel_name = 'hybrid_hgrn2_dilated_attn_moe_deepnorm'


def _layer_norm(x, gain=None, bias=None):
    xf = x.astype(jnp.float32)
    mu = xf.mean(-1, keepdims=True)
    var = jnp.square(xf - mu).mean(-1, keepdims=True)
    y = (xf - mu) * lax.rsqrt(var + LN_EPS)
    if gain is not None:
        y = y * gain.astype(jnp.float32) + bias.astype(jnp.float32)
    return y.astype(x.dtype)


def _t5_causal_bucket(dist):
    max_exact = REL_BUCKETS // 2
    n = jnp.maximum(dist, 0)
    nf = jnp.maximum(n, 1).astype(jnp.float32)
    large = max_exact + (jnp.log(nf / max_exact) / math.log(REL_MAX_DIST / max_exact)
                         * (REL_BUCKETS - max_exact)).astype(jnp.int32)
    large = jnp.minimum(large, REL_BUCKETS - 1)
    return jnp.where(n < max_exact, n, large)


def _hgrn2(q, f_logit, i, g, lb, norm_w):
    B, S, H, DK = q.shape
    DV = i.shape[-1]
    f32 = jnp.float32
    lb = lb.reshape(H, DK).astype(f32)
    z = f_logit.astype(f32)
    log_f = jnp.log(lb + (1.0 - lb) * jax.nn.sigmoid(z))
    k = (1.0 - lb) * jax.nn.sigmoid(-z)
    nc = S // HG_CHUNK

    def chunks(t):
        return t.astype(f32).reshape(B, nc, HG_CHUNK, H, t.shape[-1]).transpose(1, 0, 3, 2, 4)

    causal = jnp.tril(jnp.ones((HG_CHUNK, HG_CHUNK), bool))

    def step(state, xs):
        qc, kc, ic, lfc = xs
        b = jnp.cumsum(lfc, axis=2)
        inter = jnp.einsum('bhtd,bhde->bhte', qc * jnp.exp(b), state)
        diff = b[:, :, :, None, :] - b[:, :, None, :, :]
        decay = jnp.exp(jnp.where(causal[:, :, None], diff, -jnp.inf))
        scores = jnp.einsum('bhtd,bhsd,bhtsd->bhts', qc, kc, decay)
        intra = jnp.einsum('bhts,bhse->bhte', scores, ic)
        b_last = b[:, :, -1:]
        new_state = (jnp.exp(b_last[:, :, 0])[..., None] * state
                     + jnp.einsum('bhsd,bhse->bhde', kc * jnp.exp(b_last - b), ic))
        return new_state, inter + intra

    state0 = jnp.zeros((B, H, DK, DV), f32)
    _, o = lax.scan(step, state0, (chunks(q), chunks(k), chunks(i), chunks(log_f)))
    o = o.transpose(1, 0, 3, 2, 4).reshape(B, S, H, DV)
    o = o * lax.rsqrt(jnp.mean(o * o, -1, keepdims=True) + RMS_EPS)
    o = o.reshape(B, S, H * DV) * norm_w.astype(f32) * jax.nn.silu(g.astype(f32))
    return o.astype(q.dtype)


def _dilated_branch(q, k, v, rel_bias, window, dilation):
    B, S, H, Dh = q.shape
    W = window // dilation
    span = dilation * W
    S_pad = -(-S // span) * span
    L = S_pad // dilation
    nb = L // W

    def to_blocks(t):
        t = jnp.pad(t, ((0, 0), (0, S_pad - S), (0, 0), (0, 0)))
        t = t.reshape(B, L, dilation, H, Dh).transpose(0, 2, 3, 1, 4)
        return t.reshape(B, dilation, H, nb, W, Dh)

    def with_prev(t):
        prev = jnp.concatenate([jnp.zeros_like(t[:, :, :, :1]), t[:, :, :, :-1]], axis=3)
        return jnp.concatenate([prev, t], axis=4)

    qb = to_blocks(q)
    kk = with_prev(to_blocks(k))
    vv = with_prev(to_blocks(v))
    qi = jnp.arange(W)[:, None]
    ki = jnp.arange(2 * W)[None, :]
    m = W + qi - ki
    band = (m >= 0) & (m <= W)
    blk = jnp.arange(nb)[:, None, None]
    valid = band[None] & ((blk > 0) | (ki >= W)[None])
    bias = rel_bias[_t5_causal_bucket(m * dilation)].astype(jnp.float32).transpose(2, 0, 1)
    s = jnp.einsum('brhnqd,brhnkd->brhnqk', qb, kk).astype(jnp.float32) * (Dh ** -0.5)
    s = jnp.where(valid[None, None, None], s + bias[None, None, :, None], -jnp.inf)
    mx = s.max(-1, keepdims=True)
    p = jnp.exp(s - mx)
    l = p.sum(-1, keepdims=True)
    o = jnp.einsum('brhnqk,brhnkd->brhnqd', (p / l).astype(v.dtype), vv)
    lse = (mx + jnp.log(l))[..., 0]

    def from_blocks(t):
        extra = t.shape[5:]
        t = jnp.moveaxis(t.reshape((B, dilation, H, L) + extra), 3, 1)
        return t.reshape((B, S_pad, H) + extra)[:, :S]

    return from_blocks(o), from_blocks(lse)


def _dilated_mixture(q, k, v, rel_bias):
    B, S, _ = q.shape
    Hb = ATT_HEADS_PER_BRANCH
    outs, lses = [], []
    for g, (window, dilation) in enumerate(ATT_BRANCHES):
        lo, hi = g * Hb * ATT_HEAD_DIM, (g + 1) * Hb * ATT_HEAD_DIM
        qg = q[..., lo:hi].reshape(B, S, Hb, ATT_HEAD_DIM)
        kg = k[..., lo:hi].reshape(B, S, Hb, ATT_HEAD_DIM)
        vg = v[..., lo:hi].reshape(B, S, Hb, ATT_HEAD_DIM)
        o, lse = _dilated_branch(qg, kg, vg, rel_bias[:, g * Hb:(g + 1) * Hb], window, dilation)
        outs.append(o)
        lses.append(lse)
    wts = jax.nn.softmax(jnp.stack(lses, 0), axis=0)
    out = jnp.einsum('gbsh,gbshd->bshd', wts, jnp.stack(outs, 0).astype(jnp.float32))
    return out.reshape(B, S, ATT_OUT_WIDTH).astype(q.dtype)


def _moe(h, w_router, router_bias, w_e_gate, w_e_up, w_e_down, w_sh_gate, w_sh_up, w_sh_down):
    B, S, D = h.shape
    T = B * S
    xt = h.reshape(T, D)
    scores = jax.nn.sigmoid((xt @ w_router).astype(jnp.float32))
    biased = scores + router_bias.astype(jnp.float32)
    grp_score = lax.top_k(biased.reshape(T, N_GROUPS, N_EXPERTS // N_GROUPS), 2)[0].sum(-1)
    _, grp_idx = lax.top_k(grp_score, TOPK_GROUPS)
    grp_mask = jnp.any(grp_idx[..., None] == jnp.arange(N_GROUPS), axis=1)
    masked = jnp.where(jnp.repeat(grp_mask, N_EXPERTS // N_GROUPS, axis=1), biased, -jnp.inf)
    _, top_idx = lax.top_k(masked, TOP_K)
    top_w = jnp.take_along_axis(scores, top_idx, axis=1)
    top_w = top_w / top_w.sum(-1, keepdims=True) * ROUTED_SCALE
    A = T * TOP_K
    flat_e = top_idx.reshape(A)
    flat_w = top_w.reshape(A)
    order = jnp.argsort(flat_e)
    sorted_e = flat_e[order]
    counts = jnp.bincount(flat_e, length=N_EXPERTS)
    start = jnp.cumsum(counts) - counts
    padded = (counts + MOE_BLOCK - 1) // MOE_BLOCK * MOE_BLOCK
    pend = jnp.cumsum(padded)
    pstart = pend - padded
    dest = pstart[sorted_e] + jnp.arange(A, dtype=jnp.int32) - start[sorted_e]
    n_blocks = -(-A // MOE_BLOCK) + N_EXPERTS
    R = n_blocks * MOE_BLOCK
    row_token = jnp.zeros((R,), jnp.int32).at[dest].set((order // TOP_K).astype(jnp.int32))
    row_w = jnp.zeros((R,), jnp.float32).at[dest].set(flat_w[order])
    block_e = jnp.minimum(jnp.searchsorted(pend, jnp.arange(n_blocks) * MOE_BLOCK, side='right'),
                          N_EXPERTS - 1).astype(jnp.int32)

    def expert_block(args):
        e, tok, w = args
        xb = xt[tok]
        hb = jax.nn.silu(xb @ w_e_gate[e]) * (xb @ w_e_up[e])
        yb = hb @ w_e_down[e]
        return yb * w[:, None].astype(yb.dtype)

    y_rows = lax.map(expert_block, (block_e, row_token.reshape(n_blocks, MOE_BLOCK),
                                    row_w.reshape(n_blocks, MOE_BLOCK)))
    routed = jax.ops.segment_sum(y_rows.reshape(R, D), row_token, num_segments=T)
    shared = (jax.nn.silu(xt @ w_sh_gate) * (xt @ w_sh_up)) @ w_sh_down
    return (routed + shared).reshape(B, S, D)


def setup_inputs(seed: int = 0) -> dict:
    key = jax.random.key(seed)
    ks = jax.random.split(key, 24)
    f32 = jnp.float32

    def nrm(k, shape, scale):
        return jax.random.normal(k, shape, f32) * scale

    D = D_MODEL
    return {
        'x': nrm(ks[0], (BATCH, SEQ, D), 1.0),
        'c': nrm(ks[1], (BATCH, D), 1.0),
        'w_ada': nrm(ks[2], (DEPTH, D, 6 * D), 0.5 * D ** -0.5),
        'b_ada': nrm(ks[3], (DEPTH, 6 * D), 0.02),
        'w_in': nrm(ks[4], (DEPTH, D, IN_WIDTH), D ** -0.5),
        'hg_lower_bound': nrm(ks[5], (DEPTH + 1, HG_WIDTH), 0.5),
        'hg_norm_w': 1.0 + nrm(ks[6], (DEPTH, HG_WIDTH), 0.02),
        'rel_bias': nrm(ks[7], (REL_BUCKETS, ATT_HEADS), 0.5),
        'w_out': nrm(ks[8], (DEPTH, MIX_OUT, D), MIX_OUT ** -0.5 * DN_BETA),
        'ln1_g': 1.0 + nrm(ks[9], (DEPTH, D), 0.02),
        'ln1_b': nrm(ks[10], (DEPTH, D), 0.02),
        'w_router': nrm(ks[11], (DEPTH, D, N_EXPERTS), D ** -0.5),
        'router_bias': nrm(ks[12], (DEPTH, N_EXPERTS), 0.01),
        'w_e_gate': nrm(ks[13], (DEPTH, N_EXPERTS, D, EXPERT_DIM), D ** -0.5),
        'w_e_up': nrm(ks[14], (DEPTH, N_EXPERTS, D, EXPERT_DIM), D ** -0.5),
        'w_e_down': nrm(ks[15], (DEPTH, N_EXPERTS, EXPERT_DIM, D), EXPERT_DIM ** -0.5 * DN_BETA),
        'w_sh_gate': nrm(ks[16], (DEPTH, D, SHARED_DIM), D ** -0.5),
        'w_sh_up': nrm(ks[17], (DEPTH, D, SHARED_DIM), D ** -0.5),
        'w_sh_down': nrm(ks[18], (DEPTH, SHARED_DIM, D), SHARED_DIM ** -0.5 * DN_BETA),
        'ln2_g': 1.0 + nrm(ks[19], (DEPTH, D), 0.02),
        'ln2_b': nrm(ks[20], (DEPTH, D), 0.02),
    }


def reference(x, c, w_ada, b_ada, w_in, hg_lower_bound, hg_norm_w, rel_bias, w_out, ln1_g, ln1_b,
              w_router, router_bias, w_e_gate, w_e_up, w_e_down, w_sh_gate, w_sh_up, w_sh_down,
              ln2_g, ln2_b):
    B, S, D = x.shape
    lower_bounds = jnp.cumsum(jax.nn.softmax(hg_lower_bound.astype(jnp.float32), axis=0), axis=0)
    cond = jax.nn.silu(c)
    for l in range(DEPTH):
        mod = (cond @ w_ada[l] + b_ada[l])[:, None, :]
        sh1, sc1, g1, sh2, sc2, g2 = jnp.split(mod, 6, axis=-1)
        h = _layer_norm(x) * (1.0 + sc1) + sh1
        proj = h @ w_in[l]
        hq, hf, hi, hg, aq, ak, av = jnp.split(proj, IN_SPLITS, axis=-1)
        y_hg = _hgrn2(hq.reshape(B, S, HG_HEADS, HG_DK), hf.reshape(B, S, HG_HEADS, HG_DK),
                      hi.reshape(B, S, HG_HEADS, HG_DV), hg, lower_bounds[l], hg_norm_w[l])
        y_att = _dilated_mixture(aq, ak, av, rel_bias)
        mix = jnp.concatenate([y_hg, y_att], axis=-1) @ w_out[l]
        x = _layer_norm(DN_ALPHA * x + g1 * mix, ln1_g[l], ln1_b[l])
        h = _layer_norm(x) * (1.0 + sc2) + sh2
        ffn = _moe(h, w_router[l], router_bias[l], w_e_gate[l], w_e_up[l], w_e_down[l],
                   w_sh_gate[l], w_sh_up[l], w_sh_down[l])
        x = _layer_norm(DN_ALPHA * x + g2 * ffn, ln2_g[l], ln2_b[l])
    return x
```

```python
import math
import os
from contextlib import ExitStack
import numpy as np
import concourse.bass as bass
import concourse.mybir as mybir
from concourse.bass_utils import run_bass_kernel_spmd

F32 = mybir.dt.float32
I32 = mybir.dt.int32
U32 = mybir.dt.uint32
ALU = mybir.AluOpType
AF = mybir.ActivationFunctionType
AX = mybir.AxisListType

D = 1024
KC = 8
S_OWN = 2048
NT = 16
NE = 256
CAP = 256
NROW = NE * CAP
TRASH = NROW - 128
ALPHA = 2.0 ** 0.25
LN_EPS = 1e-5
RMS_EPS = 1e-6
NEG = -30000.0
DIL = (1, 4, 16)


class Sched:
    def __init__(self, nc, n_dma_sems=40):
        self.nc = nc
        self.eng = {'pe': nc.tensor, 'act': nc.scalar, 'dve': nc.vector, 'pool': nc.gpsimd, 'sp': nc.sync}
        self.sem = {e: nc.alloc_semaphore(name=f"c_{e}") for e in ['pe', 'act', 'dve', 'pool']}
        self.cnt = {e: 0 for e in self.sem}
        self.dsem = [nc.alloc_semaphore(name=f"d_{i}") for i in range(n_dma_sems)]
        self.dtot = [0] * n_dma_sems
        self.dnext = 0
        self.waited = {e: {} for e in self.eng}
        self.bw = {}
        self.br = {}
        self.ninst = 0

    def _wait(self, e, tok):
        sem, val = tok
        key = id(sem)
        if self.waited[e].get(key, 0) >= val:
            return
        self.eng[e].wait_ge(sem, val)
        self.waited[e][key] = val

    def _deps(self, e, r, w):
        toks = []
        for k in r:
            if k in self.bw:
                toks.append(self.bw[k])
        for k in w:
            if k in self.bw:
                toks.append(self.bw[k])
            toks.extend(self.br.get(k, []))
        for t in toks:
            if e == 'pe' and t[0] is self.sem['pe']:
                continue
            self._wait(e, t)

    def _commit(self, tok, r, w):
        for k in w:
            self.bw[k] = tok
            self.br[k] = []
        for k in r:
            lst = self.br.setdefault(k, [])
            lst.append(tok)
            if len(lst) > 24:
                best = {}
                for s, v in lst:
                    if id(s) not in best or best[id(s)][1] < v:
                        best[id(s)] = (s, v)
                self.br[k] = list(best.values())

    def op(self, e, fn, r=(), w=(), inc=True):
        self._deps(e, r, w)
        ins = fn(self.eng[e])
        self.ninst += 1
        if inc:
            ins.then_inc(self.sem[e], 1)
            self.cnt[e] += 1
            tok = (self.sem[e], self.cnt[e])
        else:
            tok = (self.sem[e], self.cnt[e] + 1)
        self._commit(tok, r, w)
        return tok

    def dma(self, q, fn, r=(), w=()):
        i = self.dnext
        self.dnext = (self.dnext + 1) % len(self.dsem)
        s = self.dsem[i]
        if self.dtot[i] > 0:
            self._wait(q, (s, self.dtot[i]))
        self._deps(q, r, w)
        ins = fn(self.eng[q])
        self.ninst += 1
        ins.then_inc(s, 16)
        self.dtot[i] += 16
        tok = (s, self.dtot[i])
        self._commit(tok, r, w)
        return tok

    def idma_batch(self, fns, r=(), w=()):
        e = 'pool'
        if not hasattr(self, 'isem'):
            self.isem = [self.nc.alloc_semaphore(name=f"i_{i}") for i in range(8)]
            self.itot = [0] * 8
        self._deps(e, r, w)
        for i, fn in enumerate(fns):
            sm = self.isem[i]
            fn(self.eng[e]).then_inc(sm, 16)
            self.itot[i] += 16
        for i in range(len(fns)):
            self.eng[e].wait_ge(self.isem[i], self.itot[i])
        ins = self.eng[e].memset(self.dummy[:, 0:1], 0.0)
        ins.then_inc(self.sem[e], 1)
        self.cnt[e] += 1
        tok = (self.sem[e], self.cnt[e])
        self._commit(tok, r, w)
        return tok

    def barrier(self):
        toks = [(self.sem[e], self.cnt[e]) for e in self.sem if self.cnt[e] > 0]
        toks += [(s, t) for s, t in zip(self.dsem, self.dtot) if t > 0]
        for e in self.eng:
            for t in toks:
                if e in self.sem and t[0] is self.sem[e]:
                    continue
                self._wait(e, t)
        self.bw = {}
        self.br = {}


def t5_bucket(dist):
    n = np.maximum(dist, 0)
    nf = np.maximum(n, 1).astype(np.float32)
    large = 16 + (np.log(nf / np.float32(16)) / np.float32(math.log(2048 / 16)) * np.float32(16)).astype(np.int32)
    large = np.minimum(large, 31)
    return np.where(n < 16, n, large)


def build(stage=3):
    nc = bass.Bass("TRN2", target_bir_lowering=False)
    S = Sched(nc)
    G = ExitStack()

    def din(name, shape, dt=F32):
        return nc.dram_tensor(name, list(shape), dt, kind="ExternalInput")

    xo = din("xo", [S_OWN, D])
    xp = din("xp", [S_OWN, D])
    flag_d = din("flag", [128, 2])
    cT_d = din("cT", [128, 8])
    w_ada = din("w_ada", [D, 6 * D])
    b_adaT = din("b_adaT", [128, 48])
    b_ada = din("b_ada", [1, 6 * D])
    w_in = din("w_in", [D, 4352])
    lbT_d = din("lbT", [128, 8])
    normw_d = din("normw", [1, 512])
    biasT_d = din("biasT", [128, 12 * 2 * 128])
    w_out = din("w_out", [768, D])
    ln1g = din("ln1g", [1, D]); ln1b = din("ln1b", [1, D])
    w_router = din("w_router", [D, NE])
    rbias = din("rbias", [1, NE])
    if stage == 3:
        w_eg = din("w_eg", [NE, D, 256]); w_eu = din("w_eu", [NE, D, 256]); w_ed = din("w_ed", [NE, 256, D])
    w_sg = din("w_sg", [D, 256]); w_su = din("w_su", [D, 256]); w_sd = din("w_sd", [256, D])
    ln2g = din("ln2g", [1, D]); ln2b = din("ln2b", [1, D])
    cst_d = din("cst", [128, 1280])
    out_d = nc.dram_tensor("out", [S_OWN, D], F32, kind="ExternalOutput")

    HT = nc.dram_tensor("HT", [KC, 128, 2 * S_OWN], F32, kind=("ExternalOutput" if stage == 1 else "Internal"))
    H2 = nc.dram_tensor("H2", [S_OWN, D], F32, kind="Internal")
    Xg = nc.dram_tensor("Xg", [NROW, D], F32, kind="Internal")
    Yg = nc.dram_tensor("Yg", [NROW, D], F32, kind="Internal")

    _uc = [0]

    def uname(name):
        _uc[0] += 1
        return f"s{_uc[0]}_{name}"

    def gsb(name, shape, dt=F32):
        return G.enter_context(nc.sbuf_tensor("s_" + name, list(shape), dt))

    ps = [G.enter_context(nc.psum_tensor(f"ps{i}", [128, 512], F32)) for i in range(8)]
    PK = [f"ps{i}" for i in range(8)]

    cst = gsb("cst", [128, 1280])
    S.dma('sp', lambda e: e.dma_start(out=cst[:], in_=cst_d.ap()), w=['cst'])
    ident = cst[:, 0:128]
    Umat = cst[:, 128:256]
    ones = cst[:, 256:384]
    cmask = cst[:, 384:448]
    pidx = cst[:, 448:449]
    limv = cst[:, 1024:1280]
    iota1 = cst[:, 512:768]
    amask = cst[:, 768:1024]
    flag = gsb("flag", [128, 2])
    dummy = gsb("dummy", [128, 2])
    S.dummy = dummy
    S.idma_batch([lambda e: e.dma_start(out=flag[:], in_=flag_d.ap())], w=['flag'])
    modT = gsb("modT", [128, 48])
    sc1p = gsb("sc1p", [128, 8])
    g1b = gsb("g1b", [128, D]); g2b = gsb("g2b", [128, D])
    sh2b = gsb("sh2b", [128, D]); sc2b = gsb("sc2b", [128, D])

    def act_copy(out, in_, r, w, scale=None):
        if scale is None:
            S.op('act', lambda e: e.activation(out=out, in_=in_, func=AF.Copy), r=r, w=w)
        else:
            S.op('act', lambda e: e.activation(out=out, in_=in_, func=AF.Copy, scale=scale), r=r, w=w)

    def dve_copy(out, in_, r, w):
        S.op('dve', lambda e: e.tensor_copy(out=out, in_=in_), r=r, w=w)

    with ExitStack() as ph:
        def sb(name, shape, dt=F32):
            return ph.enter_context(nc.sbuf_tensor(uname(name), list(shape), dt))
        cT = sb("cT", [128, 8]); condT = sb("condT", [128, 8]); condB = sb("condB", [128, 8, 128])
        badaT = sb("badaT", [128, 48])
        bb = sb("bb", [128, 512])
        wa = [sb(f"wa{i}", [128, KC, 512]) for i in range(2)]
        S.dma('sp', lambda e: e.dma_start(out=cT[:], in_=cT_d.ap()), w=['cT'])
        S.dma('sp', lambda e: e.dma_start(out=badaT[:], in_=b_adaT.ap()), w=['badaT'])
        S.op('act', lambda e: e.activation(out=condT[:], in_=cT[:], func=AF.Silu), r=['cT'], w=['condT'])
        for k in range(KC):
            S.op('dve', lambda e, k=k: e.tensor_copy(out=condB[:, k, :], in_=condT[:, k:k + 1].to_broadcast([128, 128])),
                 r=['condT'], w=['condB'])
        wv = w_ada.ap().rearrange("(k p) n -> p k n", p=128)
        bcast_dst = {4: (g1b, 0, 'g1b'), 5: (g1b, 512, 'g1b'), 6: (sh2b, 0, 'sh2b'), 7: (sh2b, 512, 'sh2b'),
                     8: (sc2b, 0, 'sc2b'), 9: (sc2b, 512, 'sc2b'), 10: (g2b, 0, 'g2b'), 11: (g2b, 512, 'g2b')}
        for j in range(12):
            wt = wa[j % 2]; wk_ = f"wa{j % 2}"
            S.dma('sp', lambda e, j=j, wt=wt: e.dma_start(out=wt[:], in_=wv[:, :, j * 512:(j + 1) * 512]), w=[wk_])
            pk = PK[j % 2]; pt = ps[j % 2]
            if j < 4:
                for sub in range(4):
                    for k in range(KC):
                        S.op('pe', lambda e, k=k, sub=sub, wt=wt, pt=pt: e.matmul(
                            pt[:, sub:sub + 1], wt[:, k, sub * 128:(sub + 1) * 128], condT[:, k:k + 1],
                            start=(k == 0), stop=(k == KC - 1)), r=[wk_, 'condT'], w=[pk], inc=(k == KC - 1))
                S.op('dve', lambda e, j=j, pt=pt: e.tensor_tensor(out=modT[:, j * 4:(j + 1) * 4], in0=pt[:, 0:4],
                                                           in1=badaT[:, j * 4:(j + 1) * 4], op=ALU.add),
                     r=[pk, 'badaT'], w=['modT'])
            else:
                dst, off, dkey = bcast_dst[j]
                S.dma('sp', lambda e, j=j: e.dma_start(out=bb[:], in_=b_ada.ap()[:, j * 512:(j + 1) * 512].partition_broadcast(128)),
                      w=['bb'])
                for k in range(KC):
                    S.op('pe', lambda e, k=k, wt=wt, pt=pt: e.matmul(pt[:, :], condB[:, k, :], wt[:, k, :],
                                                             start=(k == 0), stop=(k == KC - 1)),
                         r=[wk_, 'condB'], w=[pk], inc=(k == KC - 1))
                S.op('dve', lambda e, dst=dst, off=off, pt=pt: e.tensor_tensor(out=dst[:, off:off + 512], in0=pt[:, :],
                                                                       in1=bb[:], op=ALU.add),
                     r=[pk, 'bb'], w=[dkey])
        S.op('dve', lambda e: e.tensor_scalar(out=sc1p[:], in0=modT[:, 8:16], scalar1=1.0, scalar2=None, op0=ALU.add),
             r=['modT'], w=['sc1p'])
        S.op('dve', lambda e: e.tensor_scalar(out=sc2b[:], in0=sc2b[:], scalar1=1.0, scalar2=None, op0=ALU.add),
             r=['sc2b'], w=['sc2b'])
        S.barrier()

    def layer_norm_stats(eng_, src, key_src, st, mv, rstd, tag):
        for a in range(2):
            S.op('dve', lambda e, a=a: e.bn_stats(out=st[:, a, :], in_=src[:, a * 512:(a + 1) * 512]),
                 r=[key_src], w=[tag + 'st'])
        S.op('dve', lambda e: e.bn_aggr(out=mv[:], in_=st[:].rearrange("p a b -> p (a b)")), r=[tag + 'st'], w=[tag + 'mv'])
        S.op('act', lambda e: e.activation(out=rstd[:], in_=mv[:, 1:2], func=AF.Sqrt, bias=epsb[:, 0:1], scale=1.0),
             r=[tag + 'mv', 'epsb'], w=[tag + 'rs'])
        S.op('dve', lambda e: e.reciprocal(out=rstd[:], in_=rstd[:]), r=[tag + 'rs'], w=[tag + 'rs'])

    epsb = gsb("epsb", [128, 2])
    S.op('pool', lambda e: e.memset(epsb[:, 0:1], LN_EPS), w=['epsb'])
    S.op('pool', lambda e: e.memset(epsb[:, 1:2], RMS_EPS), w=['epsb'])

    HTv = HT.ap().rearrange("k p t -> p k t")

    with ExitStack() as ph:
        def sb(name, shape, dt=F32):
            return ph.enter_context(nc.sbuf_tensor(uname(name), list(shape), dt))
        xb = [sb(f"xb{i}", [128, D]) for i in range(3)]
        xn = [sb(f"xn{i}", [128, D]) for i in range(2)]
        hts = [sb(f"hts{i}", [128, KC, 128]) for i in range(2)]
        st = sb("st", [128, 2, 6]); mv = sb("mv", [128, 2]); rstd = sb("rstd", [128, 1])
        for tt in range(2 * NT):
            src = xp if tt < NT else xo
            row = (tt % NT) * 128
            xt = xb[tt % 3]; xk = f"xb{tt % 3}"
            S.dma('sp', lambda e, src=src, row=row, xt=xt: e.dma_start(out=xt[:], in_=src.ap()[row:row + 128, :]), w=[xk])
            layer_norm_stats('dve', xt, xk, st, mv, rstd, 'p1')
            xnt = xn[tt % 2]; xnk = f"xn{tt % 2}"
            S.op('dve', lambda e, xt=xt, xnt=xnt: e.tensor_scalar(out=xnt[:], in0=xt[:], scalar1=mv[:, 0:1], scalar2=rstd[:, 0:1],
                                                            op0=ALU.subtract, op1=ALU.mult),
                 r=[xk, 'p1mv', 'p1rs'], w=[xnk])
            ht = hts[tt % 2]; hk = f"hts{tt % 2}"
            for hb_ in range(2):
                pt = ps[(tt % 2) * 2 + hb_]; pk = PK[(tt % 2) * 2 + hb_]
                for q in range(4):
                    k = hb_ * 4 + q
                    S.op('pe', lambda e, k=k, q=q, pt=pt, xnt=xnt: e.transpose(out=pt[:, q * 128:(q + 1) * 128],
                                                                      in_=xnt[:, k * 128:(k + 1) * 128], identity=ident),
                         r=[xnk, 'cst'], w=[pk], inc=(q == 3))
                for q in range(4):
                    k = hb_ * 4 + q
                    if k % 2 == 0:
                        S.op('act', lambda e, k=k, q=q, pt=pt, ht=ht: e.activation(
                            out=ht[:, k, :], in_=pt[:, q * 128:(q + 1) * 128], func=AF.Identity,
                            bias=modT[:, k:k + 1], scale=sc1p[:, k:k + 1]), r=[pk, 'modT', 'sc1p'], w=[hk])
                    else:
                        S.op('dve', lambda e, k=k, q=q, pt=pt, ht=ht: e.tensor_scalar(
                            out=ht[:, k, :], in0=pt[:, q * 128:(q + 1) * 128], scalar1=sc1p[:, k:k + 1],
                            scalar2=modT[:, k:k + 1], op0=ALU.mult, op1=ALU.add), r=[pk, 'modT', 'sc1p'], w=[hk])
            S.dma('sp', lambda e, tt=tt, ht=ht: e.dma_start(out=HTv[:, :, tt * 128:(tt + 1) * 128], in_=ht[:]),
                  r=[hk], w=[f"HT{tt // 4}"])
        S.barrier()

    if stage == 1:
        S.barrier()
        G.close()
        return nc


    yT = gsb("yT", [128, 6, S_OWN])
    w_inv = w_in.ap().rearrange("(k p) n -> p k n", p=128)

    with ExitStack() as ph:
        def sb(name, shape, dt=F32):
            return ph.enter_context(nc.sbuf_tensor(uname(name), list(shape), dt))
        accD = sb("accD", [128, 2, S_OWN])
        biasT = sb("biasT", [128, 12, 2, 128])
        biasPH = sb("biasPH", [128, 12, 128])
        S.dma('sp', lambda e: e.dma_start(out=biasT[:].rearrange("p a b c -> p (a b c)"), in_=biasT_d.ap()), w=['biasT'])
        for h in range(12):
            S.op('dve', lambda e, h=h: e.tensor_tensor(out=biasT[:, h, :, :].rearrange("p b c -> p (b c)"),
                                                      in0=biasT[:, h, :, :].rearrange("p b c -> p (b c)"),
                                                      in1=amask, op=ALU.add), r=['biasT', 'cst'], w=['biasT'])
            S.op('dve', lambda e, h=h: e.tensor_scalar(out=biasPH[:, h, :], in0=biasT[:, h, 0, :], scalar1=flag[:, 1:2],
                                                      scalar2=None, op0=ALU.add), r=['biasT', 'flag'], w=['biasPH'])
        KT = sb("KT", [128, 2 * S_OWN]); VT = sb("VT", [128, 2 * S_OWN]); QT = sb("QT", [128, S_OWN])
        Vt = sb("Vt", [128, 32, 128])
        wqkv = [sb(f"wqkv{i}", [128, 3, KC, 128]) for i in range(1)]
        hsb = [sb(f"hsb{i}", [128, KC, 512]) for i in range(2)]
        sb1 = [sb(f"sb1_{i}", [128, 256]) for i in range(2)]
        PTs = [sb(f"PT{i}", [128, 256]) for i in range(2)]
        slab_i = 0
        blk_i = 0
        for g in range(3):
            r_ = DIL[g]; nb = 16 // r_
            for pr in range(2):
                pi = g * 2 + pr
                wt = wqkv[0]; wkey = "wqkv0"
                cols = [2048 + g * 256 + pr * 128, 2816 + g * 256 + pr * 128, 3584 + g * 256 + pr * 128]
                for j in range(3):
                    S.dma('sp', lambda e, j=j, wt=wt, cols=cols: e.dma_start(out=wt[:, j, :, :], in_=w_inv[:, :, cols[j]:cols[j] + 128]),
                          w=[wkey])
                prev_slabs = {0: [(1920, 128)], 1: [(1536, 512)], 2: [(0, 512), (512, 512), (1024, 512), (1536, 512)]}[g]
                slabs = prev_slabs + [(2048 + 512 * i, 512) for i in range(4)]
                for (c0, n) in slabs:
                    hs = hsb[slab_i % 2]; hkey = f"hsb{slab_i % 2}"; slab_i += 1
                    S.dma('sp', lambda e, hs=hs, c0=c0, n=n: e.dma_start(out=hs[:, :, 0:n], in_=HTv[:, :, c0:c0 + n]),
                          r=[f"HT{c0 // 512}"], w=[hkey])
                    own = c0 >= 2048
                    for j, (dst, dkey) in enumerate([(QT, 'QT'), (KT, 'KT'), (VT, 'VT')]):
                        if j == 0 and not own:
                            continue
                        pt = ps[j]; pk = PK[j]
                        for k in range(KC):
                            S.op('pe', lambda e, k=k, j=j, pt=pt, wt=wt, hs=hs, n=n: e.matmul(
                                pt[:, 0:n], wt[:, j, k, :], hs[:, k, 0:n], start=(k == 0), stop=(k == KC - 1)),
                                r=[wkey, hkey], w=[pk], inc=(k == KC - 1))
                        if j == 0:
                            S.op('act', lambda e, pt=pt, c0=c0, n=n: e.activation(out=QT[:, c0 - 2048:c0 - 2048 + n], in_=pt[:, 0:n],
                                                                              func=AF.Copy, scale=0.125), r=[pk], w=['QT'])
                        elif j == 1:
                            S.op('dve', lambda e, pt=pt, c0=c0, n=n: e.tensor_copy(out=KT[:, c0:c0 + n], in_=pt[:, 0:n]), r=[pk], w=['KT'])
                        else:
                            S.op('act', lambda e, pt=pt, c0=c0, n=n: e.activation(out=VT[:, c0:c0 + n], in_=pt[:, 0:n], func=AF.Copy),
                                 r=[pk], w=['VT'])
                vt_jobs = []
                for rho in range(r_):
                    vt_jobs.append((rho * nb + nb - 1, rho + r_ * 128 * (nb - 1)))
                    for n_ in range(nb):
                        vt_jobs.append((16 + rho * nb + n_, 2048 + rho + r_ * 128 * n_))
                for ji, (idx, start) in enumerate(vt_jobs):
                    pt = ps[3]; pk = PK[3]
                    q = ji % 4
                    S.op('pe', lambda e, pt=pt, q=q, start=start: e.transpose(
                        out=pt[:, q * 128:(q + 1) * 128], in_=VT[:, start:start + r_ * 127 + 1:r_], identity=ident),
                        r=['VT', 'cst'], w=[pk])
                    if ji % 2 == 0:
                        S.op('act', lambda e, pt=pt, q=q, idx=idx: e.activation(out=Vt[:, idx, :], in_=pt[:, q * 128:(q + 1) * 128], func=AF.Copy),
                             r=[pk], w=['Vt'])
                    else:
                        S.op('dve', lambda e, pt=pt, q=q, idx=idx: e.tensor_copy(out=Vt[:, idx, :], in_=pt[:, q * 128:(q + 1) * 128]),
                             r=[pk], w=['Vt'])
                for rho in range(r_ if (stage != 121 and str(g) in os.environ.get('ATT_G', '012')) else 0):
                    for n_ in range(nb):
                        qs = rho + r_ * 128 * n_
                        cidx = 16 + rho * nb + n_
                        ccol = 2048 + qs
                        if n_ > 0:
                            pidx = cidx - 1; pcol = ccol - r_ * 128
                        else:
                            pidx = rho * nb + nb - 1; pcol = rho + r_ * 128 * (nb - 1)
                        psO = ps[6 + blk_i % 2]; pko = PK[6 + blk_i % 2]
                        for hh in range(2):
                            head = g * 4 + pr * 2 + hh
                            a = (blk_i * 2 + hh) % 2
                            psS = ps[4 + a]; pks = PK[4 + a]
                            lo = hh * 64
                            qap = QT[lo:lo + 64, qs:qs + r_ * 127 + 1:r_]
                            S.op('pe', lambda e, psS=psS, lo=lo, pcol=pcol, qap=qap: e.matmul(
                                psS[:, 0:128], KT[lo:lo + 64, pcol:pcol + r_ * 127 + 1:r_], qap, start=True, stop=True),
                                r=['KT', 'QT'], w=[pks], inc=False)
                            S.op('pe', lambda e, psS=psS, lo=lo, ccol=ccol, qap=qap: e.matmul(
                                psS[:, 128:256], KT[lo:lo + 64, ccol:ccol + r_ * 127 + 1:r_], qap, start=True, stop=True),
                                r=['KT', 'QT'], w=[pks])
                            s1 = sb1[a]; s1k = f"sb1_{a}"
                            if n_ > 0:
                                S.op('dve', lambda e, s1=s1, psS=psS, head=head: e.tensor_tensor(
                                    out=s1[:], in0=psS[:, 0:256], in1=biasT[:, head, :, :].rearrange("p b c -> p (b c)"), op=ALU.add),
                                    r=[pks, 'biasT'], w=[s1k])
                            else:
                                S.op('dve', lambda e, s1=s1, psS=psS, head=head: e.tensor_tensor(
                                    out=s1[:, 0:128], in0=psS[:, 0:128], in1=biasPH[:, head, :], op=ALU.add),
                                    r=[pks, 'biasPH'], w=[s1k])
                                S.op('dve', lambda e, s1=s1, psS=psS, head=head: e.tensor_tensor(
                                    out=s1[:, 128:256], in0=psS[:, 128:256], in1=biasT[:, head, 1, :], op=ALU.add),
                                    r=[pks, 'biasT'], w=[s1k])
                            PT = PTs[a]; ptk = f"PT{a}"
                            S.op('act', lambda e, PT=PT, s1=s1: e.activation(out=PT[:], in_=s1[:], func=AF.Exp), r=[s1k], w=[ptk])
                            if 'P' in os.environ.get('ATT_SKIP', ''):
                                continue
                            S.op('pe', lambda e, psO=psO, lo=lo, pidx=pidx, PT=PT: e.matmul(
                                psO[lo:lo + 64, 0:128], Vt[:, pidx, lo:lo + 64], PT[:, 0:128], start=True, stop=False),
                                r=['Vt', ptk], w=[pko], inc=False)
                            S.op('pe', lambda e, psO=psO, lo=lo, cidx=cidx, PT=PT: e.matmul(
                                psO[lo:lo + 64, 0:128], Vt[:, cidx, lo:lo + 64], PT[:, 128:256], start=False, stop=True),
                                r=['Vt', ptk], w=[pko], inc=False)
                            S.op('pe', lambda e, psO=psO, lo=lo, PT=PT: e.matmul(
                                psO[lo:lo + 64, 128:256], ones[:, 0:64], PT[:, 0:128], start=True, stop=False),
                                r=['cst', ptk], w=[pko], inc=False)
                            S.op('pe', lambda e, psO=psO, lo=lo, PT=PT: e.matmul(
                                psO[lo:lo + 64, 128:256], ones[:, 0:64], PT[:, 128:256], start=False, stop=True),
                                r=['cst', ptk], w=[pko])
                        dN = yT[:, 4 + pr, qs:qs + r_ * 127 + 1:r_]
                        dD = accD[:, pr, qs:qs + r_ * 127 + 1:r_]
                        ykey = f"yT{4 + pr}"; dkey = f"accD{pr}"
                        if 'A' in os.environ.get('ATT_SKIP', ''):
                            pass
                        elif g == 0:
                            S.op('dve', lambda e, dN=dN, psO=psO: e.tensor_copy(out=dN, in_=psO[:, 0:128]), r=[pko], w=[ykey])
                            S.op('dve', lambda e, dD=dD, psO=psO: e.tensor_copy(out=dD, in_=psO[:, 128:256]), r=[pko], w=[dkey])
                        else:
                            S.op('dve', lambda e, dN=dN, psO=psO: e.tensor_tensor(out=dN, in0=psO[:, 0:128], in1=dN, op=ALU.add),
                                 r=[pko, ykey], w=[ykey])
                            S.op('dve', lambda e, dD=dD, psO=psO: e.tensor_tensor(out=dD, in0=psO[:, 128:256], in1=dD, op=ALU.add),
                                 r=[pko, dkey], w=[dkey])
                        blk_i += 1
        for pr in range(2):
            S.op('dve', lambda e, pr=pr: e.reciprocal(out=accD[:, pr, :], in_=accD[:, pr, :]), r=[f"accD{pr}"], w=[f"accD{pr}"])
            S.op('dve', lambda e, pr=pr: e.tensor_tensor(out=yT[:, 4 + pr, :], in0=yT[:, 4 + pr, :], in1=accD[:, pr, :], op=ALU.mult),
                 r=[f"accD{pr}", f"yT{4 + pr}"], w=[f"yT{4 + pr}"])
        S.barrier()


    if stage in (12, 121):
        G.close()
        return nc

    with ExitStack() as ph:
        def sb(name, shape, dt=F32):
            return ph.enter_context(nc.sbuf_tensor(uname(name), list(shape), dt))
        lbt = sb("lbt", [128, 8]); lb = sb("lb", [128, 4]); oml = sb("oml", [128, 4])
        S.dma('sp', lambda e: e.dma_start(out=lbt[:], in_=lbT_d.ap()), w=['lbt'])
        S.op('dve', lambda e: e.tensor_tensor(out=lb[:], in0=lbt[:, 0:4], in1=lbt[:, 4:8], op=ALU.subtract), r=['lbt'], w=['lb'])
        S.op('act', lambda e: e.activation(out=lb[:], in_=lb[:], func=AF.Sigmoid), r=['lb'], w=['lb'])
        S.op('dve', lambda e: e.tensor_scalar(out=oml[:], in0=lb[:], scalar1=-1.0, scalar2=1.0, op0=ALU.mult, op1=ALU.add), r=['lb'], w=['oml'])
        normw = sb("normw", [128, 512])
        S.dma('sp', lambda e: e.dma_start(out=normw[:], in_=normw_d.ap().partition_broadcast(128)), w=['normw'])
        wh = sb("wh", [128, 4, KC, 128])
        hsb = [sb(f"hsb{i}", [128, KC, 512]) for i in range(2)]
        state = [sb(f"st{i}", [128, 128]) for i in range(2)]
        sig = sb("sig", [128, 512]); fT = sb("fT", [128, 512]); lfT = sb("lfT", [128, 512]); kkT = sb("kkT", [128, 512])
        bT = sb("bT", [128, 512]); e1 = sb("e1", [128, 512]); kdT = sb("kdT", [128, 512])
        ebT = sb("ebT", [128, 512]); enb = sb("enb", [128, 512]); qe = sb("qe", [128, 512]); ke = sb("ke", [128, 512])
        i_sb = sb("i_sb", [128, 4, 128]); kd_sb = sb("kd_sb", [128, 4, 128]); g_sb = sb("g_sb", [128, 4, 128])
        sT = [sb(f"sT{i}", [128, 64]) for i in range(2)]
        ssq = sb("ssq", [128, 1]); rs = sb("rs", [128, 1]); junk = sb("junk", [128, 128])
        t1 = sb("t1", [128, 128]); yv = sb("yv", [128, 128]); eblast = sb("eblast", [128, 8])
        slab_i = 0
        for h in range(4):
            for j in range(4):
                S.dma('sp', lambda e, j=j, h=h: e.dma_start(out=wh[:, j, :, :], in_=w_inv[:, :, j * 512 + h * 128:j * 512 + (h + 1) * 128]),
                      w=['wh'])
            S.op('pool', lambda e: e.memset(state[0][:], 0.0), w=['st0'])
            ci = 0
            for si in range(8):
                own = si >= 4
                c0 = si * 512
                hs = hsb[slab_i % 2]; hkey = f"hsb{slab_i % 2}"; slab_i += 1
                S.dma('sp', lambda e, hs=hs, c0=c0: e.dma_start(out=hs[:], in_=HTv[:, :, c0:c0 + 512]), r=[f"HT{si}"], w=[hkey])
                for k in range(KC):
                    S.op('pe', lambda e, k=k, hs=hs: e.matmul(ps[0][:, :], wh[:, 1, k, :], hs[:, k, :], start=(k == 0), stop=(k == KC - 1)),
                         r=['wh', hkey], w=['ps0'], inc=(k == KC - 1))
                S.op('act', lambda e: e.activation(out=sig[:], in_=ps[0][:, :], func=AF.Sigmoid), r=['ps0'], w=['sig'])
                S.op('dve', lambda e, h=h: e.tensor_scalar(out=fT[:], in0=sig[:], scalar1=oml[:, h:h + 1], scalar2=lb[:, h:h + 1],
                                                          op0=ALU.mult, op1=ALU.add), r=['sig', 'oml', 'lb'], w=['fT'])
                S.op('act', lambda e: e.activation(out=lfT[:], in_=fT[:], func=AF.Ln), r=['fT'], w=['lfT'])
                S.op('pool', lambda e: e.tensor_scalar(out=kkT[:], in0=fT[:], scalar1=-1.0, scalar2=1.0, op0=ALU.mult, op1=ALU.add),
                     r=['fT'], w=['kkT'])
                for c in range(8):
                    S.op('dve', lambda e, c=c: e.tensor_tensor_scan(out=bT[:, c * 64:(c + 1) * 64], data0=ones[:, 0:64],
                                                                   data1=lfT[:, c * 64:(c + 1) * 64], initial=0.0,
                                                                   op0=ALU.mult, op1=ALU.add), r=['lfT', 'cst'], w=['bT'])
                blast = bT[:].rearrange("p (c s) -> p c s", s=64)[:, :, 63]
                S.op('act', lambda e: e.activation(out=eblast[:], in_=blast, func=AF.Exp), r=['bT'], w=['eblast'])
                for c in range(8):
                    S.op('act', lambda e, c=c: e.activation(out=e1[:, c * 64:(c + 1) * 64], in_=bT[:, c * 64:(c + 1) * 64], func=AF.Exp,
                                                           bias=bT[:, c * 64 + 63:c * 64 + 64], scale=-1.0), r=['bT'], w=['e1'])
                S.op('pool', lambda e: e.tensor_tensor(out=kdT[:], in0=e1[:], in1=kkT[:], op=ALU.mult), r=['e1', 'kkT'], w=['kdT'])
                for j in range(4):
                    for k in range(KC):
                        S.op('pe', lambda e, k=k, j=j, hs=hs: e.matmul(ps[2][:, j * 128:(j + 1) * 128], hs[:, k, j * 128:(j + 1) * 128],
                                                                    wh[:, 2, k, :], start=(k == 0), stop=(k == KC - 1)),
                             r=['wh', hkey], w=['ps2'], inc=(k == KC - 1))
                if own:
                    S.op('act', lambda e: e.activation(out=i_sb[:].rearrange("p a b -> p (a b)"), in_=ps[2][:, :], func=AF.Copy),
                         r=['ps2'], w=['i_sb'])
                else:
                    S.op('dve', lambda e: e.tensor_scalar(out=i_sb[:].rearrange("p a b -> p (a b)"), in0=ps[2][:, :], scalar1=flag[:, 0:1],
                                                        scalar2=None, op0=ALU.mult), r=['ps2', 'flag'], w=['i_sb'])
                for j in range(4):
                    S.op('pe', lambda e, j=j: e.transpose(out=ps[4][:, j * 128:(j + 1) * 128], in_=kdT[:, j * 128:(j + 1) * 128], identity=ident),
                         r=['kdT', 'cst'], w=['ps4'], inc=(j == 3))
                S.op('dve', lambda e: e.tensor_copy(out=kd_sb[:].rearrange("p a b -> p (a b)"), in_=ps[4][:, :]), r=['ps4'], w=['kd_sb'])
                if own:
                    for k in range(KC):
                        S.op('pe', lambda e, k=k, hs=hs: e.matmul(ps[1][:, :], wh[:, 0, k, :], hs[:, k, :], start=(k == 0), stop=(k == KC - 1)),
                             r=['wh', hkey], w=['ps1'], inc=(k == KC - 1))
                    S.op('act', lambda e: e.activation(out=ebT[:], in_=bT[:], func=AF.Exp), r=['bT'], w=['ebT'])
                    S.op('dve', lambda e: e.tensor_tensor(out=qe[:], in0=ps[1][:, :], in1=ebT[:], op=ALU.mult), r=['ps1', 'ebT'], w=['qe'])
                    S.op('act', lambda e: e.activation(out=enb[:], in_=bT[:], func=AF.Exp, scale=-1.0), r=['bT'], w=['enb'])
                    S.op('pool', lambda e: e.tensor_tensor(out=ke[:], in0=kkT[:], in1=enb[:], op=ALU.mult), r=['kkT', 'enb'], w=['ke'])
                    for j in range(4):
                        for k in range(KC):
                            S.op('pe', lambda e, k=k, j=j, hs=hs: e.matmul(ps[3][:, j * 128:(j + 1) * 128], hs[:, k, j * 128:(j + 1) * 128],
                                                                        wh[:, 3, k, :], start=(k == 0), stop=(k == KC - 1)),
                                 r=['wh', hkey], w=['ps3'], inc=(k == KC - 1))
                    S.op('act', lambda e: e.activation(out=g_sb[:].rearrange("p a b -> p (a b)"), in_=ps[3][:, :], func=AF.Silu),
                         r=['ps3'], w=['g_sb'])
                for c in range(8):
                    j = c // 2; b0 = (c % 2) * 64
                    cs = slice(c * 64, (c + 1) * 64)
                    cur = state[ci % 2]; nxt = state[(ci + 1) % 2]
                    ck = f"st{ci % 2}"; nk = f"st{(ci + 1) % 2}"
                    ci += 1
                    o6 = ps[6][:, (j % 2) * 128:(j % 2) * 128 + 128]; o6k = "ps6"
                    if own:
                        sTt = sT[c % 2]; sTk = f"sT{c % 2}"
                        S.op('pe', lambda e, b0=b0, cs=cs: e.matmul(ps[5][b0:b0 + 64, 0:64], ke[:, cs], qe[:, cs], start=True, stop=True),
                             r=['ke', 'qe'], w=['ps5'])
                        S.op('dve', lambda e, b0=b0, sTt=sTt: e.tensor_tensor(out=sTt[b0:b0 + 64, :], in0=ps[5][b0:b0 + 64, 0:64],
                                                                            in1=cmask[b0:b0 + 64, :], op=ALU.mult),
                             r=['ps5', 'cst'], w=[sTk])
                        S.op('pe', lambda e, b0=b0, cs=cs, cur=cur, o6=o6: e.matmul(o6[b0:b0 + 64, :], qe[:, cs], cur[:, :], start=True, stop=False),
                             r=['qe', ck], w=[o6k], inc=False)
                        S.op('pe', lambda e, b0=b0, sTt=sTt, j=j, o6=o6: e.matmul(o6[b0:b0 + 64, :], sTt[b0:b0 + 64, :], i_sb[b0:b0 + 64, j, :],
                                                                            start=False, stop=True), r=[sTk, 'i_sb'], w=[o6k])
                    u7 = ps[7][:, (c % 2) * 128:(c % 2) * 128 + 128]; u7k = "ps7"
                    S.op('pe', lambda e, b0=b0, j=j, u7=u7: e.matmul(u7, kd_sb[b0:b0 + 64, j, :], i_sb[b0:b0 + 64, j, :], start=True, stop=True),
                         r=['kd_sb', 'i_sb'], w=[u7k])
                    S.op('dve', lambda e, cur=cur, nxt=nxt, c=c, u7=u7: e.scalar_tensor_tensor(
                        out=nxt[:], in0=cur[:], scalar=eblast[:, c:c + 1], in1=u7, op0=ALU.mult, op1=ALU.add),
                        r=[ck, 'eblast', u7k], w=[nk])
                    if own and c % 2 == 1:
                        S.op('act', lambda e, o6=o6: e.activation(out=junk[:], in_=o6, func=AF.Square, accum_out=ssq[:]), r=[o6k], w=['junk', 'ssq'])
                        S.op('act', lambda e: e.activation(out=rs[:], in_=ssq[:], func=AF.Sqrt, bias=epsb[:, 1:2], scale=1.0 / 128.0),
                             r=['ssq', 'epsb'], w=['rs'])
                        S.op('dve', lambda e: e.reciprocal(out=rs[:], in_=rs[:]), r=['rs'], w=['rs'])
                        S.op('dve', lambda e, o6=o6, h=h: e.scalar_tensor_tensor(out=t1[:], in0=o6, scalar=rs[:, 0:1],
                                                                               in1=normw[:, h * 128:(h + 1) * 128], op0=ALU.mult, op1=ALU.mult),
                             r=[o6k, 'rs', 'normw'], w=['t1'])
                        S.op('pool', lambda e, j=j: e.tensor_tensor(out=yv[:], in0=t1[:], in1=g_sb[:, j, :], op=ALU.mult), r=['t1', 'g_sb'], w=['yv'])
                        S.op('pe', lambda e: e.transpose(out=ps[5][:, 256:384], in_=yv[:], identity=ident), r=['yv', 'cst'], w=['ps5'])
                        tok0 = (si - 4) * 512 + j * 128
                        S.op('act', lambda e, h=h, tok0=tok0: e.activation(out=yT[:, h, tok0:tok0 + 128], in_=ps[5][:, 256:384], func=AF.Copy),
                             r=['ps5'], w=[f"yT{h}"])
        S.barrier()

    if stage == 13:
        G.close()
        return nc

    x1 = gsb("x1", [128, NT, D])
    with ExitStack() as ph:
        def sb(name, shape, dt=F32):
            return ph.enter_context(nc.sbuf_tensor(uname(name), list(shape), dt))
        wo = sb("wo", [128, 6, D])
        S.dma('sp', lambda e: e.dma_start(out=wo[:], in_=w_out.ap().rearrange("(k p) n -> p k n", p=128)), w=['wo'])
        lg = sb("lg", [128, D]); lbb = sb("lbb", [128, D])
        S.dma('sp', lambda e: e.dma_start(out=lg[:], in_=ln1g.ap().partition_broadcast(128)), w=['lg'])
        S.dma('sp', lambda e: e.dma_start(out=lbb[:], in_=ln1b.ap().partition_broadcast(128)), w=['lbb'])
        xb = [sb(f"xb{i}", [128, D]) for i in range(2)]
        tb = [sb(f"tb{i}", [128, D]) for i in range(2)]
        st = sb("st", [128, 2, 6]); mv = sb("mv", [128, 2]); rstd = sb("rstd", [128, 1])
        for tt in range(NT):
            xt = xb[tt % 2]; xk = f"xb{tt % 2}"
            S.dma('sp', lambda e, tt=tt, xt=xt: e.dma_start(out=xt[:], in_=xo.ap()[tt * 128:(tt + 1) * 128, :]), w=[xk])
            t = tb[tt % 2]; tk = f"tb{tt % 2}"
            for hf in range(2):
                pi = (tt % 2) * 2 + hf
                for k in range(6):
                    S.op('pe', lambda e, k=k, hf=hf, tt=tt, pi=pi: e.matmul(ps[pi][:, :], yT[:, k, tt * 128:(tt + 1) * 128],
                                                                     wo[:, k, hf * 512:(hf + 1) * 512], start=(k == 0), stop=(k == 5)),
                         r=['wo'] + [f"yT{k}"], w=[PK[pi]], inc=(k == 5))
                S.op('dve', lambda e, hf=hf, pi=pi, t=t: e.tensor_tensor(out=t[:, hf * 512:(hf + 1) * 512], in0=ps[pi][:, :],
                                                                   in1=g1b[:, hf * 512:(hf + 1) * 512], op=ALU.mult),
                     r=[PK[pi], 'g1b'], w=[tk])
            S.op('dve', lambda e, xt=xt, t=t: e.scalar_tensor_tensor(out=t[:], in0=xt[:], scalar=ALPHA, in1=t[:], op0=ALU.mult, op1=ALU.add),
                 r=[xk, tk], w=[tk])
            layer_norm_stats('dve', t, tk, st, mv, rstd, 'p4')
            S.op('dve', lambda e, tt=tt, t=t: e.tensor_scalar(out=x1[:, tt, :], in0=t[:], scalar1=mv[:, 0:1], scalar2=rstd[:, 0:1],
                                                        op0=ALU.subtract, op1=ALU.mult), r=[tk, 'p4mv', 'p4rs'], w=[f"x1_{tt}"])
            S.op('pool', lambda e, tt=tt: e.tensor_tensor(out=x1[:, tt, :], in0=x1[:, tt, :], in1=lg[:], op=ALU.mult), r=[f"x1_{tt}", 'lg'], w=[f"x1_{tt}"])
            S.op('pool', lambda e, tt=tt: e.tensor_tensor(out=x1[:, tt, :], in0=x1[:, tt, :], in1=lbb[:], op=ALU.add), r=[f"x1_{tt}", 'lbb'], w=[f"x1_{tt}"])
            if stage == 2:
                S.dma('sp', lambda e, tt=tt: e.dma_start(out=out_d.ap()[tt * 128:(tt + 1) * 128, :], in_=x1[:, tt, :]), r=[f"x1_{tt}"], w=['out'])
        S.barrier()

    if stage == 2:
        G.close()
        return nc


    dest_all = gsb("dest_all", [128, NT, 8], I32)
    wk_all = gsb("wk_all", [128, NT, 8])
    H2v = H2.ap()
    bc_reg = nc.gpsimd.to_reg(NROW - 1)
    with ExitStack() as ph:
        def sb(name, shape, dt=F32):
            return ph.enter_context(nc.sbuf_tensor(uname(name), list(shape), dt))
        wr = sb("wr", [128, KC, NE])
        S.dma('sp', lambda e: e.dma_start(out=wr[:], in_=w_router.ap().rearrange("(k p) n -> p k n", p=128)), w=['wr'])
        rbb = sb("rbb", [128, NE])
        S.dma('sp', lambda e: e.dma_start(out=rbb[:], in_=rbias.ap().partition_broadcast(128)), w=['rbb'])
        cum = sb("cum", [128, NE])
        S.op('pool', lambda e: e.memset(cum[:], 0.0), w=['cum'])
        h2b = [sb(f"h2_{i}", [128, D]) for i in range(2)]
        xn2 = sb("xn2", [128, D]); h2T = sb("h2T", [128, KC, 128])
        st = sb("st", [128, 2, 6]); mv = sb("mv", [128, 2]); rstd = sb("rstd", [128, 1])
        scores = sb("scores", [128, NE]); biased = sb("biased", [128, NE]); m8 = sb("m8", [128, 8, 8])
        gs = sb("gs", [128, 8]); g8 = sb("g8", [128, 8]); gmask = sb("gmask", [128, 8]); negoff = sb("negoff", [128, 8])
        masked = sb("masked", [128, NE]); t8 = sb("t8", [128, 8]); sel = sb("sel", [128, NE]); ssel = sb("ssel", [128, NE])
        ssum = sb("ssum", [128, 1]); Wc = sb("Wc", [128, NE]); sel2 = sb("sel2", [128, NE]); Vp = sb("Vp", [128, NE])
        d8 = sb("d8", [128, 8]); junk2 = sb("junk2", [128, NE]); dtr = sb("dtr", [128, 8])
        for tt in range(NT):
            xk = f"x1_{tt}"
            layer_norm_stats('dve', x1[:, tt, :], xk, st, mv, rstd, 'p5')
            S.op('dve', lambda e, tt=tt: e.tensor_scalar(out=xn2[:], in0=x1[:, tt, :], scalar1=mv[:, 0:1], scalar2=rstd[:, 0:1],
                                                        op0=ALU.subtract, op1=ALU.mult), r=[xk, 'p5mv', 'p5rs'], w=['xn2'])
            h2 = h2b[tt % 2]; hk = f"h2_{tt % 2}"
            S.op('pool', lambda e, h2=h2: e.tensor_tensor(out=h2[:], in0=xn2[:], in1=sc2b[:], op=ALU.mult), r=['xn2', 'sc2b'], w=[hk])
            S.op('pool', lambda e, h2=h2: e.tensor_tensor(out=h2[:], in0=h2[:], in1=sh2b[:], op=ALU.add), r=[hk, 'sh2b'], w=[hk])
            S.dma('sp', lambda e, tt=tt, h2=h2: e.dma_start(out=H2v[tt * 128:(tt + 1) * 128, :], in_=h2[:]), r=[hk], w=['H2'])
            for hb_ in range(2):
                for q in range(4):
                    k = hb_ * 4 + q
                    S.op('pe', lambda e, k=k, q=q, hb_=hb_, h2=h2: e.transpose(out=ps[hb_][:, q * 128:(q + 1) * 128],
                                                                        in_=h2[:, k * 128:(k + 1) * 128], identity=ident),
                         r=[hk, 'cst'], w=[PK[hb_]], inc=(q == 3))
            S.op('act', lambda e: e.activation(out=h2T[:, 0:4, :].rearrange("p a b -> p (a b)"), in_=ps[0][:, :], func=AF.Copy), r=['ps0'], w=['h2Ta'])
            S.op('dve', lambda e: e.tensor_copy(out=h2T[:, 4:8, :].rearrange("p a b -> p (a b)"), in_=ps[1][:, :]), r=['ps1'], w=['h2Tb'])
            for k in range(KC):
                S.op('pe', lambda e, k=k: e.matmul(ps[2][:, 0:NE], h2T[:, k, :], wr[:, k, :], start=(k == 0), stop=(k == KC - 1)),
                     r=['h2Ta', 'h2Tb', 'wr'], w=['ps2'], inc=(k == KC - 1))
            S.op('act', lambda e: e.activation(out=scores[:], in_=ps[2][:, 0:NE], func=AF.Sigmoid), r=['ps2'], w=['scores'])
            S.op('dve', lambda e: e.tensor_tensor(out=biased[:], in0=scores[:], in1=rbb[:], op=ALU.add), r=['scores', 'rbb'], w=['biased'])
            for gi in range(8):
                S.op('dve', lambda e, gi=gi: e.max(out=m8[:, gi, :], in_=biased[:, gi * 32:(gi + 1) * 32]), r=['biased'], w=['m8'])
            S.op('dve', lambda e: e.tensor_tensor(out=gs[:], in0=m8[:, :, 0], in1=m8[:, :, 1], op=ALU.add), r=['m8'], w=['gs'])
            S.op('dve', lambda e: e.max(out=g8[:], in_=gs[:]), r=['gs'], w=['g8'])
            S.op('dve', lambda e: e.tensor_scalar(out=gmask[:], in0=gs[:], scalar1=g8[:, 3:4], scalar2=None, op0=ALU.is_ge),
                 r=['gs', 'g8'], w=['gmask'])
            S.op('dve', lambda e: e.tensor_scalar(out=negoff[:], in0=gmask[:], scalar1=10.0, scalar2=-10.0, op0=ALU.mult, op1=ALU.add),
                 r=['gmask'], w=['negoff'])
            for gi in range(8):
                S.op('dve', lambda e, gi=gi: e.tensor_scalar(out=masked[:, gi * 32:(gi + 1) * 32], in0=biased[:, gi * 32:(gi + 1) * 32],
                                                            scalar1=gmask[:, gi:gi + 1], scalar2=negoff[:, gi:gi + 1],
                                                            op0=ALU.mult, op1=ALU.add), r=['biased', 'gmask', 'negoff'], w=['masked'])
            S.op('dve', lambda e: e.max(out=t8[:], in_=masked[:]), r=['masked'], w=['t8'])
            S.op('dve', lambda e: e.tensor_scalar(out=sel[:], in0=masked[:], scalar1=t8[:, 7:8], scalar2=None, op0=ALU.is_ge),
                 r=['masked', 't8'], w=['sel'])
            S.op('dve', lambda e: e.scalar_tensor_tensor(out=ssel[:], in0=scores[:], scalar=1.0, in1=sel[:], op0=ALU.mult, op1=ALU.mult,
                                                        accum_out=ssum[:]), r=['scores', 'sel'], w=['ssel', 'ssum'])
            S.op('dve', lambda e: e.reciprocal(out=ssum[:], in_=ssum[:]), r=['ssum'], w=['ssum'])
            S.op('dve', lambda e: e.tensor_scalar(out=Wc[:], in0=ssel[:], scalar1=ssum[:, 0:1], scalar2=2.5, op0=ALU.mult, op1=ALU.mult),
                 r=['ssel', 'ssum'], w=['Wc'])
            S.op('pe', lambda e: e.matmul(ps[3][:, 0:NE], Umat, sel[:], start=True, stop=False), r=['sel', 'cst'], w=['ps3'], inc=False)
            S.op('pe', lambda e: e.matmul(ps[3][:, 0:NE], ones, cum[:], start=False, stop=True), r=['cum', 'cst'], w=['ps3'])
            S.op('dve', lambda e: e.tensor_tensor(out=Vp[:], in0=ps[3][:, 0:NE], in1=iota1, op=ALU.add), r=['ps3', 'cst'], w=['Vp'])
            S.op('dve', lambda e: e.tensor_tensor(out=sel2[:], in0=Vp[:], in1=limv, op=ALU.is_lt), r=['Vp', 'cst'], w=['sel2'])
            S.op('dve', lambda e: e.tensor_tensor(out=sel2[:], in0=sel2[:], in1=sel[:], op=ALU.mult), r=['sel2', 'sel'], w=['sel2'])
            S.op('dve', lambda e: e.tensor_tensor(out=Vp[:], in0=Vp[:], in1=sel2[:], op=ALU.mult), r=['Vp', 'sel2'], w=['Vp'])
            S.op('dve', lambda e: e.tensor_tensor(out=Wc[:], in0=Wc[:], in1=sel2[:], op=ALU.mult), r=['Wc', 'sel2'], w=['Wc'])
            S.op('pool', lambda e: e.tensor_tensor(out=cum[:], in0=cum[:], in1=sel[:], op=ALU.add), r=['cum', 'sel'], w=['cum'])
            S.op('dve', lambda e: e.max(out=d8[:], in_=Vp[:]), r=['Vp'], w=['d8'])
            S.op('dve', lambda e: e.tensor_scalar(out=dtr[:], in0=d8[:], scalar1=0.0, scalar2=pidx, op0=ALU.is_equal, op1=ALU.mult),
                 r=['d8', 'cst'], w=['dtr'])
            S.op('dve', lambda e, tt=tt: e.scalar_tensor_tensor(out=dest_all[:, tt, :], in0=d8[:], scalar=-1.0, in1=dtr[:],
                                                                op0=ALU.add, op1=ALU.add), r=['d8', 'dtr'], w=[f"dest{tt}"])
            for k in range(8):
                S.op('dve', lambda e, k=k, tt=tt: e.scalar_tensor_tensor(out=junk2[:], in0=Vp[:], scalar=d8[:, k:k + 1], in1=Wc[:],
                                                                        op0=ALU.is_equal, op1=ALU.mult,
                                                                        accum_out=wk_all[:, tt, k:k + 1]),
                     r=['Vp', 'd8', 'Wc'], w=['junk2', f"wk{tt}"])
            for half in range(0 if 'D' in os.environ.get('MOE_SKIP', '') else 2):
                S.idma_batch([
                    (lambda e, k=k, tt=tt, h2=h2: e.indirect_dma_start(
                        out=Xg.ap(), out_offset=bass.IndirectOffsetOnAxis(ap=dest_all[:, tt, k:k + 1], axis=0),
                        in_=h2[:], in_offset=None, bounds_check=bc_reg, oob_is_err=False))
                    for k in range(half * 4, half * 4 + 4)], r=[hk, f"dest{tt}"], w=['Xg'])
        S.barrier()

    if stage == 25:
        dbgt = gsb("dbgt", [128, NT, 16])
        S.op('dve', lambda e: e.tensor_copy(out=dbgt[:, :, 0:8], in_=dest_all[:]), w=['dbgt'])
        S.op('dve', lambda e: e.tensor_copy(out=dbgt[:, :, 8:16], in_=wk_all[:]), w=['dbgt'])
        S.dma('sp', lambda e: e.dma_start(out=out_d.ap()[0:128, 0:256], in_=dbgt[:].rearrange("p a b -> p (a b)")), r=['dbgt'], w=['out'])
        S.barrier()
        G.close()
        return nc

    with ExitStack() as ph:
        def sb(name, shape, dt=F32):
            return ph.enter_context(nc.sbuf_tensor(uname(name), list(shape), dt))
        wgu = [sb(f"wgu{i}", [128, KC, 512]) for i in range(2)]
        wd = [sb(f"wd{i}", [128, 2, D]) for i in range(2)]
        xg = [sb(f"xg{i}", [128, D]) for i in range(2)]
        yg = [sb(f"yg{i}", [128, D]) for i in range(2)]
        xgT = sb("xgT", [128, KC, 128]); sg = sb("sg", [128, 256]); hb = sb("hb", [128, 256]); hbT = sb("hbT", [128, 2, 128])
        NEX = int(os.environ.get('MOE_NEX', NE))
        S.op('pool', lambda e: e.memset(yg[0][:], 0.0), w=['yg0'])
        S.dma('sp', lambda e: e.dma_start(out=Yg.ap()[TRASH:TRASH + 128, :], in_=yg[0][:]), r=['yg0'], w=['Yg'])

        def load(e_):
            i = e_ % 2
            S.dma('sp', lambda e: e.dma_start(out=wgu[i][:, :, 0:256], in_=w_eg.ap()[e_].rearrange("(k p) f -> p k f", p=128)), w=[f"wgu{i}"])
            S.dma('sp', lambda e: e.dma_start(out=wgu[i][:, :, 256:512], in_=w_eu.ap()[e_].rearrange("(k p) f -> p k f", p=128)), w=[f"wgu{i}"])
            S.dma('sp', lambda e: e.dma_start(out=wd[i][:], in_=w_ed.ap()[e_].rearrange("(k p) n -> p k n", p=128)), w=[f"wd{i}"])
        NBLK = CAP // 128
        load(0)
        for eb in range(NEX * NBLK - (1 if NEX == NE else 0)):
            e_ = eb // NBLK; blk = eb % NBLK
            if blk == 0 and e_ + 1 < NEX:
                load(e_ + 1)
            i = e_ % 2
            xi = eb % 2
            S.dma('sp', lambda e, eb=eb, xi=xi: e.dma_start(out=xg[xi][:], in_=Xg.ap()[eb * 128:(eb + 1) * 128, :]), w=[f"xg{xi}"])
            for hb_ in range(2):
                for q in range(4):
                    k = hb_ * 4 + q
                    S.op('pe', lambda e, k=k, q=q, hb_=hb_, xi=xi: e.transpose(out=ps[hb_][:, q * 128:(q + 1) * 128],
                                                                        in_=xg[xi][:, k * 128:(k + 1) * 128], identity=ident),
                         r=[f"xg{xi}", 'cst'], w=[PK[hb_]], inc=(q == 3))
            S.op('act', lambda e: e.activation(out=xgT[:, 0:4, :].rearrange("p a b -> p (a b)"), in_=ps[0][:, :], func=AF.Copy), r=['ps0'], w=['xgTa'])
            S.op('dve', lambda e: e.tensor_copy(out=xgT[:, 4:8, :].rearrange("p a b -> p (a b)"), in_=ps[1][:, :]), r=['ps1'], w=['xgTb'])
            for k in range(KC):
                S.op('pe', lambda e, k=k, i=i: e.matmul(ps[2][:, :], xgT[:, k, :], wgu[i][:, k, :], start=(k == 0), stop=(k == KC - 1)),
                     r=['xgTa', 'xgTb', f"wgu{i}"], w=['ps2'], inc=(k == KC - 1))
            S.op('act', lambda e: e.activation(out=sg[:], in_=ps[2][:, 0:256], func=AF.Silu), r=['ps2'], w=['sg'])
            S.op('dve', lambda e: e.tensor_tensor(out=hb[:], in0=ps[2][:, 256:512], in1=sg[:], op=ALU.mult), r=['ps2', 'sg'], w=['hb'])
            for fc in range(2):
                S.op('pe', lambda e, fc=fc: e.transpose(out=ps[3][:, fc * 128:(fc + 1) * 128], in_=hb[:, fc * 128:(fc + 1) * 128], identity=ident),
                     r=['hb', 'cst'], w=['ps3'], inc=(fc == 1))
            S.op('dve', lambda e: e.tensor_copy(out=hbT[:].rearrange("p a b -> p (a b)"), in_=ps[3][:, 0:256]), r=['ps3'], w=['hbT'])
            for hf in range(2):
                for fc in range(2):
                    S.op('pe', lambda e, fc=fc, hf=hf, i=i: e.matmul(ps[4 + hf][:, :], hbT[:, fc, :], wd[i][:, fc, hf * 512:(hf + 1) * 512],
                                                               start=(fc == 0), stop=(fc == 1)), r=['hbT', f"wd{i}"], w=[PK[4 + hf]], inc=(fc == 1))
            S.op('act', lambda e, xi=xi: e.activation(out=yg[xi][:, 0:512], in_=ps[4][:, :], func=AF.Copy), r=['ps4'], w=[f"yg{xi}"])
            S.op('dve', lambda e, xi=xi: e.tensor_copy(out=yg[xi][:, 512:1024], in_=ps[5][:, :]), r=['ps5'], w=[f"yg{xi}"])
            S.dma('sp', lambda e, xi=xi, eb=eb: e.dma_start(out=Yg.ap()[eb * 128:(eb + 1) * 128, :], in_=yg[xi][:]), r=[f"yg{xi}"], w=['Yg'])
        S.barrier()

    with ExitStack() as ph:
        def sb(name, shape, dt=F32):
            return ph.enter_context(nc.sbuf_tensor(uname(name), list(shape), dt))
        wsgu = sb("wsgu", [128, KC, 512]); wsd = sb("wsd", [128, 2, D])
        S.dma('sp', lambda e: e.dma_start(out=wsgu[:, :, 0:256], in_=w_sg.ap().rearrange("(k p) f -> p k f", p=128)), w=['wsgu'])
        S.dma('sp', lambda e: e.dma_start(out=wsgu[:, :, 256:512], in_=w_su.ap().rearrange("(k p) f -> p k f", p=128)), w=['wsgu'])
        S.dma('sp', lambda e: e.dma_start(out=wsd[:], in_=w_sd.ap().rearrange("(k p) n -> p k n", p=128)), w=['wsd'])
        lg2 = sb("lg2", [128, D]); lb2 = sb("lb2", [128, D])
        S.dma('sp', lambda e: e.dma_start(out=lg2[:], in_=ln2g.ap().partition_broadcast(128)), w=['lg2'])
        S.dma('sp', lambda e: e.dma_start(out=lb2[:], in_=ln2b.ap().partition_broadcast(128)), w=['lb2'])
        h2r = sb("h2r", [128, D]); h2T = sb("h2T", [128, KC, 128]); sg = sb("sg", [128, 256]); hs = sb("hs", [128, 256])
        hsT = sb("hsT", [128, 2, 128]); acc = sb("acc", [128, D]); ot = sb("ot", [128, D])
        gb = [sb(f"gb{i}", [128, D]) for i in range(4)]
        st = sb("st", [128, 2, 6]); mv = sb("mv", [128, 2]); rstd = sb("rstd", [128, 1])
        for i in range(4):
            S.op('pool', lambda e, i=i: e.memset(gb[i][:], 0.0), w=[f"gb{i}"])
        for tt in range(NT):
            S.dma('sp', lambda e, tt=tt: e.dma_start(out=h2r[:], in_=H2v[tt * 128:(tt + 1) * 128, :]), w=['h2r'])
            for hb_ in range(2):
                for q in range(4):
                    k = hb_ * 4 + q
                    S.op('pe', lambda e, k=k, q=q, hb_=hb_: e.transpose(out=ps[hb_][:, q * 128:(q + 1) * 128],
                                                                 in_=h2r[:, k * 128:(k + 1) * 128], identity=ident),
                         r=['h2r', 'cst'], w=[PK[hb_]], inc=(q == 3))
            S.op('act', lambda e: e.activation(out=h2T[:, 0:4, :].rearrange("p a b -> p (a b)"), in_=ps[0][:, :], func=AF.Copy), r=['ps0'], w=['h2Ta'])
            S.op('dve', lambda e: e.tensor_copy(out=h2T[:, 4:8, :].rearrange("p a b -> p (a b)"), in_=ps[1][:, :]), r=['ps1'], w=['h2Tb'])
            for k in range(KC):
                S.op('pe', lambda e, k=k: e.matmul(ps[2][:, :], h2T[:, k, :], wsgu[:, k, :], start=(k == 0), stop=(k == KC - 1)),
                     r=['h2Ta', 'h2Tb', 'wsgu'], w=['ps2'], inc=(k == KC - 1))
            S.op('act', lambda e: e.activation(out=sg[:], in_=ps[2][:, 0:256], func=AF.Silu), r=['ps2'], w=['sg'])
            S.op('dve', lambda e: e.tensor_tensor(out=hs[:], in0=ps[2][:, 256:512], in1=sg[:], op=ALU.mult), r=['ps2', 'sg'], w=['hs'])
            for fc in range(2):
                S.op('pe', lambda e, fc=fc: e.transpose(out=ps[3][:, fc * 128:(fc + 1) * 128], in_=hs[:, fc * 128:(fc + 1) * 128], identity=ident),
                     r=['hs', 'cst'], w=['ps3'], inc=(fc == 1))
            S.op('dve', lambda e: e.tensor_copy(out=hsT[:].rearrange("p a b -> p (a b)"), in_=ps[3][:, 0:256]), r=['ps3'], w=['hsT'])
            for hf in range(2):
                for fc in range(2):
                    S.op('pe', lambda e, fc=fc, hf=hf: e.matmul(ps[4 + hf][:, :], hsT[:, fc, :], wsd[:, fc, hf * 512:(hf + 1) * 512],
                                                          start=(fc == 0), stop=(fc == 1)), r=['hsT', 'wsd'], w=[PK[4 + hf]], inc=(fc == 1))
            S.op('dve', lambda e: e.tensor_copy(out=acc[:, 0:512], in_=ps[4][:, :]), r=['ps4'], w=['acc'])
            S.op('dve', lambda e: e.tensor_copy(out=acc[:, 512:1024], in_=ps[5][:, :]), r=['ps5'], w=['acc'])
            for half in range(2):
                S.idma_batch([
                    (lambda e, k=k, tt=tt, j=j: e.indirect_dma_start(
                        out=gb[j][:], out_offset=None, in_=Yg.ap(),
                        in_offset=bass.IndirectOffsetOnAxis(ap=dest_all[:, tt, k:k + 1], axis=0),
                        bounds_check=bc_reg, oob_is_err=False))
                    for j, k in enumerate(range(half * 4, half * 4 + 4))], r=['Yg', f"dest{tt}"], w=[f"gb{j}" for j in range(4)])
                for j, k in enumerate(range(half * 4, half * 4 + 4)):
                    S.op('dve', lambda e, j=j, k=k, tt=tt: e.scalar_tensor_tensor(out=acc[:], in0=gb[j][:], scalar=wk_all[:, tt, k:k + 1],
                                                                              in1=acc[:], op0=ALU.mult, op1=ALU.add),
                         r=[f"gb{j}", f"wk{tt}", 'acc'], w=['acc'])
            S.op('pool', lambda e: e.tensor_tensor(out=acc[:], in0=acc[:], in1=g2b[:], op=ALU.mult), r=['acc', 'g2b'], w=['acc'])
            S.op('dve', lambda e, tt=tt: e.scalar_tensor_tensor(out=acc[:], in0=x1[:, tt, :], scalar=ALPHA, in1=acc[:], op0=ALU.mult, op1=ALU.add),
                 r=[f"x1_{tt}", 'acc'], w=['acc'])
            layer_norm_stats('dve', acc, 'acc', st, mv, rstd, 'p7')
            S.op('dve', lambda e: e.tensor_scalar(out=ot[:], in0=acc[:], scalar1=mv[:, 0:1], scalar2=rstd[:, 0:1],
                                                op0=ALU.subtract, op1=ALU.mult), r=['acc', 'p7mv', 'p7rs'], w=['ot'])
            S.op('pool', lambda e: e.tensor_tensor(out=ot[:], in0=ot[:], in1=lg2[:], op=ALU.mult), r=['ot', 'lg2'], w=['ot'])
            S.op('pool', lambda e: e.tensor_tensor(out=ot[:], in0=ot[:], in1=lb2[:], op=ALU.add), r=['ot', 'lb2'], w=['ot'])
            S.dma('sp', lambda e, tt=tt: e.dma_start(out=out_d.ap()[tt * 128:(tt + 1) * 128, :], in_=ot[:]), r=['ot'], w=['out'])
        S.barrier()
    G.close()
    return nc


def make_consts():
    c = np.zeros((128, 1280), np.float32)
    c[:, 0:128] = np.eye(128, dtype=np.float32)
    tp = np.arange(128)[:, None]; tf = np.arange(128)[None, :]
    c[:, 128:256] = (tp < tf).astype(np.float32)
    c[:, 256:384] = 1.0
    sp_ = (np.arange(128) % 64)[:, None]; t_ = np.arange(64)[None, :]
    c[:, 384:448] = (sp_ <= t_).astype(np.float32)
    c[:, 512:768] = (np.arange(256) * CAP + 1)[None, :].astype(np.float32)
    c[:, 448] = (TRASH + 1 + np.arange(128)).astype(np.float32)
    lim = np.arange(256) * CAP + 1 + CAP
    lim[255] -= 128
    c[:, 1024:1280] = lim[None, :].astype(np.float32)
    ki = np.arange(128)[:, None]; qi = np.arange(128)[None, :]
    c[:, 768:896] = np.where(qi <= ki, 0.0, NEG)
    c[:, 896:1024] = np.where(ki <= qi, 0.0, NEG)
    return c


def make_bias_idx():
    ki = np.arange(128)[:, None]; qi = np.arange(128)[None, :]
    idx = np.zeros((3, 2, 128, 128), np.int64)
    for g, r in enumerate(DIL):
        idx[g, 0] = t5_bucket(np.clip(128 + qi - ki, 0, 10 ** 6) * r)
        idx[g, 1] = t5_bucket(np.clip(qi - ki, 0, 10 ** 6) * r)
    return idx


def prep_inputs(inp):
    f = lambda a: np.ascontiguousarray(np.asarray(a, dtype=np.float32))
    x = f(inp['x']); c = f(inp['c'])
    cst = make_consts()
    idx = make_bias_idx()
    rb = f(inp['rel_bias'])
    biasT = np.zeros((128, 12, 2, 128), np.float32)
    for h in range(12):
        g = h // 4
        for pc in range(2):
            biasT[:, h, pc, :] = rb[idx[g, pc], h]
    biasT = biasT.reshape(128, -1)
    hlb = f(inp['hg_lower_bound'])
    lbT = np.concatenate([hlb[0].reshape(4, 128).T, hlb[1].reshape(4, 128).T], axis=1)
    shared = {
        'w_ada': f(inp['w_ada'][0]), 'b_adaT': f(inp['b_ada'][0]).reshape(48, 128).T.copy(), 'b_ada': f(inp['b_ada']),
        'w_in': f(inp['w_in'][0]), 'lbT': np.ascontiguousarray(lbT), 'normw': f(inp['hg_norm_w']),
        'biasT': biasT, 'w_out': f(inp['w_out'][0]), 'ln1g': f(inp['ln1_g']), 'ln1b': f(inp['ln1_b']),
        'w_router': f(inp['w_router'][0]), 'rbias': f(inp['router_bias']),
        'w_eg': f(inp['w_e_gate'][0]), 'w_eu': f(inp['w_e_up'][0]), 'w_ed': f(inp['w_e_down'][0]),
        'w_sg': f(inp['w_sh_gate'][0]), 'w_su': f(inp['w_sh_up'][0]), 'w_sd': f(inp['w_sh_down'][0]),
        'ln2g': f(inp['ln2_g']), 'ln2b': f(inp['ln2_b']), 'cst': cst,
    }
    maps = []
    for core in range(8):
        b, half = core // 2, core % 2
        m = dict(shared)
        m['xo'] = x[b, half * S_OWN:(half + 1) * S_OWN]
        m['xp'] = x[b, 0:S_OWN] if half == 1 else np.zeros((S_OWN, D), np.float32)
        fl = np.zeros((128, 2), np.float32)
        fl[:, 0] = float(half); fl[:, 1] = 0.0 if half == 1 else NEG
        m['flag'] = fl
        m['cT'] = np.ascontiguousarray(c[b].reshape(8, 128).T)
        maps.append(m)
    return maps


_NC_CACHE = {}


def kernel(**inputs):
    maps = prep_inputs(inputs)
    nc = build(3)
    names = set(a.memorylocations[0].name for a in nc.allocations
                if isinstance(a, mybir.MemoryLocationSet) and a.kind == "ExternalInput")
    maps = [{k: v for k, v in m.items() if k in names} for m in maps]
    res = run_bass_kernel_spmd(nc, maps, core_ids=list(range(8)))
    out = np.zeros((4, 2 * S_OWN, D), np.float32)
    for core in range(8):
        b, half = core // 2, core % 2
        out[b, half * S_OWN:(half + 1) * S_OWN] = res.results[core]['out']
    return out
```

```python
import math
import os
from contextlib import ExitStack
import numpy as np
import concourse.bass as bass
import concourse.mybir as mybir
from concourse.bass_utils import run_bass_kernel_spmd

F32 = mybir.dt.float32
F32R = mybir.dt.float32r
I32 = mybir.dt.int32
U32 = mybir.dt.uint32
ALU = mybir.AluOpType
AF = mybir.ActivationFunctionType
AX = mybir.AxisListType

D = 1024
KC = 8
S_OWN = 2048
NT = 16
NE = 256
CAP = 256
NROW = NE * CAP
TRASH = NROW - 128
ALPHA = 2.0 ** 0.25
LN_EPS = 1e-5
RMS_EPS = 1e-6
NEG = -30000.0
DIL = (1, 4, 16)


class Sched:
    def __init__(self, nc, n_dma_sems=40):
        self.nc = nc
        self.eng = {'pe': nc.tensor, 'act': nc.scalar, 'dve': nc.vector, 'pool': nc.gpsimd, 'sp': nc.sync}
        self.sem = {e: nc.alloc_semaphore(name=f"c_{e}") for e in ['pe', 'act', 'dve', 'pool']}
        self.cnt = {e: 0 for e in self.sem}
        self.dsem = [nc.alloc_semaphore(name=f"d_{i}") for i in range(n_dma_sems)]
        self.dtot = [0] * n_dma_sems
        self.dnext = 0
        self.waited = {e: {} for e in self.eng}
        self.bw = {}
        self.br = {}
        self.ninst = 0

    def _wait(self, e, tok):
        sem, val = tok
        key = id(sem)
        if self.waited[e].get(key, 0) >= val:
            return
        self.eng[e].wait_ge(sem, val)
        self.waited[e][key] = val

    def _deps(self, e, r, w):
        toks = []
        for k in r:
            if k in self.bw:
                toks.append(self.bw[k])
        for k in w:
            if k in self.bw:
                toks.append(self.bw[k])
            toks.extend(self.br.get(k, []))
        for t in toks:
            if e == 'pe' and t[0] is self.sem['pe']:
                continue
            self._wait(e, t)

    def _commit(self, tok, r, w):
        for k in w:
            self.bw[k] = tok
            self.br[k] = []
        for k in r:
            lst = self.br.setdefault(k, [])
            lst.append(tok)
            if len(lst) > 24:
                best = {}
                for s, v in lst:
                    if id(s) not in best or best[id(s)][1] < v:
                        best[id(s)] = (s, v)
                self.br[k] = list(best.values())

    def op(self, e, fn, r=(), w=(), inc=True):
        self._deps(e, r, w)
        ins = fn(self.eng[e])
        self.ninst += 1
        if inc:
            ins.then_inc(self.sem[e], 1)
            self.cnt[e] += 1
            tok = (self.sem[e], self.cnt[e])
        else:
            tok = (self.sem[e], self.cnt[e] + 1)
        self._commit(tok, r, w)
        return tok

    def dma(self, q, fn, r=(), w=()):
        i = self.dnext
        self.dnext = (self.dnext + 1) % len(self.dsem)
        s = self.dsem[i]
        if self.dtot[i] > 0:
            self._wait(q, (s, self.dtot[i]))
        self._deps(q, r, w)
        ins = fn(self.eng[q])
        self.ninst += 1
        ins.then_inc(s, 16)
        self.dtot[i] += 16
        tok = (s, self.dtot[i])
        self._commit(tok, r, w)
        return tok

    def idma_batch(self, fns, r=(), w=()):
        e = 'pool'
        if not hasattr(self, 'isem'):
            self.isem = [self.nc.alloc_semaphore(name=f"i_{i}") for i in range(8)]
            self.itot = [0] * 8
        self._deps(e, r, w)
        for i, fn in enumerate(fns):
            sm = self.isem[i]
            fn(self.eng[e]).then_inc(sm, 16)
            self.itot[i] += 16
        for i in range(len(fns)):
            self.eng[e].wait_ge(self.isem[i], self.itot[i])
        ins = self.eng[e].memset(self.dummy[:, 0:1], 0.0)
        ins.then_inc(self.sem[e], 1)
        self.cnt[e] += 1
        tok = (self.sem[e], self.cnt[e])
        self._commit(tok, r, w)
        return tok

    def barrier(self):
        toks = [(self.sem[e], self.cnt[e]) for e in self.sem if self.cnt[e] > 0]
        toks += [(s, t) for s, t in zip(self.dsem, self.dtot) if t > 0]
        for e in self.eng:
            for t in toks:
                if e in self.sem and t[0] is self.sem[e]:
                    continue
                self._wait(e, t)
        self.bw = {}
        self.br = {}


def t5_bucket(dist):
    n = np.maximum(dist, 0)
    nf = np.maximum(n, 1).astype(np.float32)
    large = 16 + (np.log(nf / np.float32(16)) / np.float32(math.log(2048 / 16)) * np.float32(16)).astype(np.int32)
    large = np.minimum(large, 31)
    return np.where(n < 16, n, large)


def build(stage=3):
    nc = bass.Bass("TRN2", target_bir_lowering=False)
    S = Sched(nc)
    G = ExitStack()

    def din(name, shape, dt=F32):
        return nc.dram_tensor(name, list(shape), dt, kind="ExternalInput")

    xo = din("xo", [S_OWN, D])
    xp = din("xp", [S_OWN, D])
    flag_d = din("flag", [128, 2])
    cT_d = din("cT", [128, 8])
    w_ada = din("w_ada", [D, 6 * D])
    b_adaT = din("b_adaT", [128, 48])
    b_ada = din("b_ada", [1, 6 * D])
    w_in = din("w_in", [D, 4352])
    lbT_d = din("lbT", [128, 8])
    normw_d = din("normw", [1, 512])
    biasT_d = din("biasT", [128, 12 * 2 * 128])
    w_out = din("w_out", [768, D])
    ln1g = din("ln1g", [1, D]); ln1b = din("ln1b", [1, D])
    w_router = din("w_router", [D, NE])
    rbias = din("rbias", [1, NE])
    if stage == 3:
        w_eg = din("w_eg", [NE, D, 256]); w_eu = din("w_eu", [NE, D, 256]); w_ed = din("w_ed", [NE, 256, D])
    w_sg = din("w_sg", [D, 256]); w_su = din("w_su", [D, 256]); w_sd = din("w_sd", [256, D])
    ln2g = din("ln2g", [1, D]); ln2b = din("ln2b", [1, D])
    cst_d = din("cst", [128, 1280])
    out_d = nc.dram_tensor("out", [S_OWN, D], F32, kind="ExternalOutput")

    HT = nc.dram_tensor("HT", [KC, 128, 2 * S_OWN], F32, kind=("ExternalOutput" if stage == 1 else "Internal"))
    H2 = nc.dram_tensor("H2", [S_OWN, D], F32, kind="Internal")
    Xg = nc.dram_tensor("Xg", [NROW, D], F32, kind="Internal")
    Yg = nc.dram_tensor("Yg", [NROW, D], F32, kind="Internal")

    _uc = [0]

    def uname(name):
        _uc[0] += 1
        return f"s{_uc[0]}_{name}"

    def gsb(name, shape, dt=F32):
        return G.enter_context(nc.sbuf_tensor("s_" + name, list(shape), dt))

    ps = [G.enter_context(nc.psum_tensor(f"ps{i}", [128, 512], F32)) for i in range(8)]
    PK = [f"ps{i}" for i in range(8)]

    cst = gsb("cst", [128, 1280])
    S.dma('sp', lambda e: e.dma_start(out=cst[:], in_=cst_d.ap()), w=['cst'])
    ident = cst[:, 0:128]
    Umat = cst[:, 128:256]
    ones = cst[:, 256:384]
    cmask = cst[:, 384:448]
    pidx = cst[:, 448:449]
    limv = cst[:, 1024:1280]
    iota1 = cst[:, 512:768]
    amask = cst[:, 768:1024]
    flag = gsb("flag", [128, 2])
    dummy = gsb("dummy", [128, 2])
    S.dummy = dummy
    S.idma_batch([lambda e: e.dma_start(out=flag[:], in_=flag_d.ap())], w=['flag'])
    modT = gsb("modT", [128, 48])
    sc1p = gsb("sc1p", [128, 8])
    g1b = gsb("g1b", [128, D]); g2b = gsb("g2b", [128, D])
    sh2b = gsb("sh2b", [128, D]); sc2b = gsb("sc2b", [128, D])

    def act_copy(out, in_, r, w, scale=None):
        if scale is None:
            S.op('act', lambda e: e.activation(out=out, in_=in_, func=AF.Copy), r=r, w=w)
        else:
            S.op('act', lambda e: e.activation(out=out, in_=in_, func=AF.Copy, scale=scale), r=r, w=w)

    def dve_copy(out, in_, r, w):
        S.op('dve', lambda e: e.tensor_copy(out=out, in_=in_), r=r, w=w)

    with ExitStack() as ph:
        def sb(name, shape, dt=F32):
            return ph.enter_context(nc.sbuf_tensor(uname(name), list(shape), dt))
        cT = sb("cT", [128, 8]); condT = sb("condT", [128, 8]); condB = sb("condB", [128, 8, 128])
        badaT = sb("badaT", [128, 48])
        bb = sb("bb", [128, 512])
        wa = [sb(f"wa{i}", [128, KC, 512]) for i in range(2)]
        S.dma('sp', lambda e: e.dma_start(out=cT[:], in_=cT_d.ap()), w=['cT'])
        S.dma('sp', lambda e: e.dma_start(out=badaT[:], in_=b_adaT.ap()), w=['badaT'])
        S.op('act', lambda e: e.activation(out=condT[:], in_=cT[:], func=AF.Silu), r=['cT'], w=['condT'])
        for k in range(KC):
            S.op('dve', lambda e, k=k: e.tensor_copy(out=condB[:, k, :], in_=condT[:, k:k + 1].to_broadcast([128, 128])),
                 r=['condT'], w=['condB'])
        wv = w_ada.ap().rearrange("(k p) n -> p k n", p=128)
        bcast_dst = {4: (g1b, 0, 'g1b'), 5: (g1b, 512, 'g1b'), 6: (sh2b, 0, 'sh2b'), 7: (sh2b, 512, 'sh2b'),
                     8: (sc2b, 0, 'sc2b'), 9: (sc2b, 512, 'sc2b'), 10: (g2b, 0, 'g2b'), 11: (g2b, 512, 'g2b')}
        for j in range(12):
            wt = wa[j % 2]; wk_ = f"wa{j % 2}"
            S.dma('sp', lambda e, j=j, wt=wt: e.dma_start(out=wt[:], in_=wv[:, :, j * 512:(j + 1) * 512]), w=[wk_])
            pk = PK[j % 2]; pt = ps[j % 2]
            if j < 4:
                for sub in range(4):
                    for k in range(KC):
                        S.op('pe', lambda e, k=k, sub=sub, wt=wt, pt=pt: e.matmul(
                            pt[:, sub:sub + 1], wt[:, k, sub * 128:(sub + 1) * 128], condT[:, k:k + 1],
                            start=(k == 0), stop=(k == KC - 1)), r=[wk_, 'condT'], w=[pk], inc=(k == KC - 1))
                S.op('dve', lambda e, j=j, pt=pt: e.tensor_tensor(out=modT[:, j * 4:(j + 1) * 4], in0=pt[:, 0:4],
                                                           in1=badaT[:, j * 4:(j + 1) * 4], op=ALU.add),
                     r=[pk, 'badaT'], w=['modT'])
            else:
                dst, off, dkey = bcast_dst[j]
                S.dma('sp', lambda e, j=j: e.dma_start(out=bb[:], in_=b_ada.ap()[:, j * 512:(j + 1) * 512].partition_broadcast(128)),
                      w=['bb'])
                for k in range(KC):
                    S.op('pe', lambda e, k=k, wt=wt, pt=pt: e.matmul(pt[:, :], condB[:, k, :], wt[:, k, :],
                                                             start=(k == 0), stop=(k == KC - 1)),
                         r=[wk_, 'condB'], w=[pk], inc=(k == KC - 1))
                S.op('dve', lambda e, dst=dst, off=off, pt=pt: e.tensor_tensor(out=dst[:, off:off + 512], in0=pt[:, :],
                                                                       in1=bb[:], op=ALU.add),
                     r=[pk, 'bb'], w=[dkey])
        S.op('dve', lambda e: e.tensor_scalar(out=sc1p[:], in0=modT[:, 8:16], scalar1=1.0, scalar2=None, op0=ALU.add),
             r=['modT'], w=['sc1p'])
        S.op('dve', lambda e: e.tensor_scalar(out=sc2b[:], in0=sc2b[:], scalar1=1.0, scalar2=None, op0=ALU.add),
             r=['sc2b'], w=['sc2b'])
        S.barrier()

    def layer_norm_stats(eng_, src, key_src, st, mv, rstd, tag):
        for a in range(2):
            S.op('dve', lambda e, a=a: e.bn_stats(out=st[:, a, :], in_=src[:, a * 512:(a + 1) * 512]),
                 r=[key_src], w=[tag + 'st'])
        S.op('dve', lambda e: e.bn_aggr(out=mv[:], in_=st[:].rearrange("p a b -> p (a b)")), r=[tag + 'st'], w=[tag + 'mv'])
        S.op('act', lambda e: e.activation(out=rstd[:], in_=mv[:, 1:2], func=AF.Sqrt, bias=epsb[:, 0:1], scale=1.0),
             r=[tag + 'mv', 'epsb'], w=[tag + 'rs'])
        S.op('dve', lambda e: e.reciprocal(out=rstd[:], in_=rstd[:]), r=[tag + 'rs'], w=[tag + 'rs'])

    epsb = gsb("epsb", [128, 2])
    S.op('pool', lambda e: e.memset(epsb[:, 0:1], LN_EPS), w=['epsb'])
    S.op('pool', lambda e: e.memset(epsb[:, 1:2], RMS_EPS), w=['epsb'])

    HTv = HT.ap().rearrange("k p t -> p k t")

    with ExitStack() as ph:
        def sb(name, shape, dt=F32):
            return ph.enter_context(nc.sbuf_tensor(uname(name), list(shape), dt))
        xb = [sb(f"xb{i}", [128, D]) for i in range(3)]
        xn = [sb(f"xn{i}", [128, D]) for i in range(2)]
        hts = [sb(f"hts{i}", [128, KC, 128]) for i in range(2)]
        st = sb("st", [128, 2, 6]); mv = sb("mv", [128, 2]); rstd = sb("rstd", [128, 1])
        for tt in range(2 * NT):
            src = xp if tt < NT else xo
            row = (tt % NT) * 128
            xt = xb[tt % 3]; xk = f"xb{tt % 3}"
            S.dma('sp', lambda e, src=src, row=row, xt=xt: e.dma_start(out=xt[:], in_=src.ap()[row:row + 128, :]), w=[xk])
            layer_norm_stats('dve', xt, xk, st, mv, rstd, 'p1')
            xnt = xn[tt % 2]; xnk = f"xn{tt % 2}"
            S.op('dve', lambda e, xt=xt, xnt=xnt: e.tensor_scalar(out=xnt[:], in0=xt[:], scalar1=mv[:, 0:1], scalar2=rstd[:, 0:1],
                                                            op0=ALU.subtract, op1=ALU.mult),
                 r=[xk, 'p1mv', 'p1rs'], w=[xnk])
            ht = hts[tt % 2]; hk = f"hts{tt % 2}"
            for hb_ in range(2):
                pt = ps[(tt % 2) * 2 + hb_]; pk = PK[(tt % 2) * 2 + hb_]
                for q in range(4):
                    k = hb_ * 4 + q
                    S.op('pe', lambda e, k=k, q=q, pt=pt, xnt=xnt: e.transpose(out=pt[:, q * 128:(q + 1) * 128],
                                                                      in_=xnt[:, k * 128:(k + 1) * 128], identity=ident),
                         r=[xnk, 'cst'], w=[pk], inc=(q == 3))
                for q in range(4):
                    k = hb_ * 4 + q
                    if k % 2 == 0:
                        S.op('act', lambda e, k=k, q=q, pt=pt, ht=ht: e.activation(
                            out=ht[:, k, :], in_=pt[:, q * 128:(q + 1) * 128], func=AF.Identity,
                            bias=modT[:, k:k + 1], scale=sc1p[:, k:k + 1]), r=[pk, 'modT', 'sc1p'], w=[hk])
                    else:
                        S.op('dve', lambda e, k=k, q=q, pt=pt, ht=ht: e.tensor_scalar(
                            out=ht[:, k, :], in0=pt[:, q * 128:(q + 1) * 128], scalar1=sc1p[:, k:k + 1],
                            scalar2=modT[:, k:k + 1], op0=ALU.mult, op1=ALU.add), r=[pk, 'modT', 'sc1p'], w=[hk])
            S.dma('sp', lambda e, tt=tt, ht=ht: e.dma_start(out=HTv[:, :, tt * 128:(tt + 1) * 128], in_=ht[:]),
                  r=[hk], w=[f"HT{tt // 4}"])
        S.barrier()

    if stage == 1:
        S.barrier()
        G.close()
        return nc


    yT = gsb("yT", [128, 6, S_OWN])
    w_inv = w_in.ap().rearrange("(k p) n -> p k n", p=128)

    with ExitStack() as ph:
        def sb(name, shape, dt=F32):
            return ph.enter_context(nc.sbuf_tensor(uname(name), list(shape), dt))
        accD = sb("accD", [128, 2, S_OWN])
        biasT = sb("biasT", [128, 12, 2, 128])
        biasPH = sb("biasPH", [128, 12, 128])
        S.dma('sp', lambda e: e.dma_start(out=biasT[:].rearrange("p a b c -> p (a b c)"), in_=biasT_d.ap()), w=['biasT'])
        for h in range(12):
            S.op('dve', lambda e, h=h: e.tensor_tensor(out=biasT[:, h, :, :].rearrange("p b c -> p (b c)"),
                                                      in0=biasT[:, h, :, :].rearrange("p b c -> p (b c)"),
                                                      in1=amask, op=ALU.add), r=['biasT', 'cst'], w=['biasT'])
            S.op('dve', lambda e, h=h: e.tensor_scalar(out=biasPH[:, h, :], in0=biasT[:, h, 0, :], scalar1=flag[:, 1:2],
                                                      scalar2=None, op0=ALU.add), r=['biasT', 'flag'], w=['biasPH'])
        KT = sb("KT", [128, 2 * S_OWN]); VT = sb("VT", [128, 2 * S_OWN]); QT = sb("QT", [128, S_OWN])
        Vt = sb("Vt", [128, 32, 128])
        wqkv = [sb(f"wqkv{i}", [128, 3, KC, 128]) for i in range(1)]
        hsb = [sb(f"hsb{i}", [128, KC, 512]) for i in range(2)]
        sb1 = [sb(f"sb1_{i}", [128, 256]) for i in range(2)]
        PTs = [sb(f"PT{i}", [128, 256]) for i in range(2)]
        slab_i = 0
        blk_i = 0
        for g in range(3):
            r_ = DIL[g]; nb = 16 // r_
            for pr in range(2):
                pi = g * 2 + pr
                wt = wqkv[0]; wkey = "wqkv0"
                cols = [2048 + g * 256 + pr * 128, 2816 + g * 256 + pr * 128, 3584 + g * 256 + pr * 128]
                for j in range(3):
                    S.dma('sp', lambda e, j=j, wt=wt, cols=cols: e.dma_start(out=wt[:, j, :, :], in_=w_inv[:, :, cols[j]:cols[j] + 128]),
                          w=[wkey])
                prev_slabs = {0: [(1920, 128)], 1: [(1536, 512)], 2: [(0, 512), (512, 512), (1024, 512), (1536, 512)]}[g]
                slabs = prev_slabs + [(2048 + 512 * i, 512) for i in range(4)]
                for (c0, n) in slabs:
                    hs = hsb[slab_i % 2]; hkey = f"hsb{slab_i % 2}"; slab_i += 1
                    S.dma('sp', lambda e, hs=hs, c0=c0, n=n: e.dma_start(out=hs[:, :, 0:n], in_=HTv[:, :, c0:c0 + n]),
                          r=[f"HT{c0 // 512}"], w=[hkey])
                    own = c0 >= 2048
                    for j, (dst, dkey) in enumerate([(QT, 'QT'), (KT, 'KT'), (VT, 'VT')]):
                        if j == 0 and not own:
                            continue
                        pt = ps[j]; pk = PK[j]
                        for k in range(KC):
                            S.op('pe', lambda e, k=k, j=j, pt=pt, wt=wt, hs=hs, n=n: e.matmul(
                                pt[:, 0:n], wt[:, j, k, :], hs[:, k, 0:n], start=(k == 0), stop=(k == KC - 1)),
                                r=[wkey, hkey], w=[pk], inc=(k == KC - 1))
                        if j == 0:
                            S.op('act', lambda e, pt=pt, c0=c0, n=n: e.activation(out=QT[:, c0 - 2048:c0 - 2048 + n], in_=pt[:, 0:n],
                                                                              func=AF.Copy, scale=0.125), r=[pk], w=['QT'])
                        elif j == 1:
                            S.op('dve', lambda e, pt=pt, c0=c0, n=n: e.tensor_copy(out=KT[:, c0:c0 + n], in_=pt[:, 0:n]), r=[pk], w=['KT'])
                        else:
                            S.op('act', lambda e, pt=pt, c0=c0, n=n: e.activation(out=VT[:, c0:c0 + n], in_=pt[:, 0:n], func=AF.Copy),
                                 r=[pk], w=['VT'])
                vt_jobs = []
                for rho in range(r_):
                    vt_jobs.append((rho * nb + nb - 1, rho + r_ * 128 * (nb - 1)))
                    for n_ in range(nb):
                        vt_jobs.append((16 + rho * nb + n_, 2048 + rho + r_ * 128 * n_))
                for ji, (idx, start) in enumerate(vt_jobs):
                    pt = ps[3]; pk = PK[3]
                    q = ji % 4
                    S.op('pe', lambda e, pt=pt, q=q, start=start: e.transpose(
                        out=pt[:, q * 128:(q + 1) * 128], in_=VT[:, start:start + r_ * 127 + 1:r_], identity=ident),
                        r=['VT', 'cst'], w=[pk])
                    if ji % 2 == 0:
                        S.op('act', lambda e, pt=pt, q=q, idx=idx: e.activation(out=Vt[:, idx, :], in_=pt[:, q * 128:(q + 1) * 128], func=AF.Copy),
                             r=[pk], w=['Vt'])
                    else:
                        S.op('dve', lambda e, pt=pt, q=q, idx=idx: e.tensor_copy(out=Vt[:, idx, :], in_=pt[:, q * 128:(q + 1) * 128]),
                             r=[pk], w=['Vt'])
                for rho in range(r_ if (stage != 121 and str(g) in os.environ.get('ATT_G', '012')) else 0):
                    for n_ in range(nb):
                        qs = rho + r_ * 128 * n_
                        cidx = 16 + rho * nb + n_
                        ccol = 2048 + qs
                        if n_ > 0:
                            pidx = cidx - 1; pcol = ccol - r_ * 128
                        else:
                            pidx = rho * nb + nb - 1; pcol = rho + r_ * 128 * (nb - 1)
                        psO = ps[6 + blk_i % 2]; pko = PK[6 + blk_i % 2]
                        for hh in range(2):
                            head = g * 4 + pr * 2 + hh
                            a = (blk_i * 2 + hh) % 2
                            psS = ps[4 + a]; pks = PK[4 + a]
                            lo = hh * 64
                            qap = QT[lo:lo + 64, qs:qs + r_ * 127 + 1:r_]
                            S.op('pe', lambda e, psS=psS, lo=lo, pcol=pcol, qap=qap: e.matmul(
                                psS[:, 0:128], KT[lo:lo + 64, pcol:pcol + r_ * 127 + 1:r_], qap, start=True, stop=True),
                                r=['KT', 'QT'], w=[pks], inc=False)
                            S.op('pe', lambda e, psS=psS, lo=lo, ccol=ccol, qap=qap: e.matmul(
                                psS[:, 128:256], KT[lo:lo + 64, ccol:ccol + r_ * 127 + 1:r_], qap, start=True, stop=True),
                                r=['KT', 'QT'], w=[pks])
                            s1 = sb1[a]; s1k = f"sb1_{a}"
                            if n_ > 0:
                                S.op('dve', lambda e, s1=s1, psS=psS, head=head: e.tensor_tensor(
                                    out=s1[:], in0=psS[:, 0:256], in1=biasT[:, head, :, :].rearrange("p b c -> p (b c)"), op=ALU.add),
                                    r=[pks, 'biasT'], w=[s1k])
                            else:
                                S.op('dve', lambda e, s1=s1, psS=psS, head=head: e.tensor_tensor(
                                    out=s1[:, 0:128], in0=psS[:, 0:128], in1=biasPH[:, head, :], op=ALU.add),
                                    r=[pks, 'biasPH'], w=[s1k])
                                S.op('dve', lambda e, s1=s1, psS=psS, head=head: e.tensor_tensor(
                                    out=s1[:, 128:256], in0=psS[:, 128:256], in1=biasT[:, head, 1, :], op=ALU.add),
                                    r=[pks, 'biasT'], w=[s1k])
                            PT = PTs[a]; ptk = f"PT{a}"
                            S.op('act', lambda e, PT=PT, s1=s1: e.activation(out=PT[:], in_=s1[:], func=AF.Exp), r=[s1k], w=[ptk])
                            if 'P' in os.environ.get('ATT_SKIP', ''):
                                continue
                            S.op('pe', lambda e, psO=psO, lo=lo, pidx=pidx, PT=PT: e.matmul(
                                psO[lo:lo + 64, 0:128], Vt[:, pidx, lo:lo + 64], PT[:, 0:128], start=True, stop=False),
                                r=['Vt', ptk], w=[pko], inc=False)
                            S.op('pe', lambda e, psO=psO, lo=lo, cidx=cidx, PT=PT: e.matmul(
                                psO[lo:lo + 64, 0:128], Vt[:, cidx, lo:lo + 64], PT[:, 128:256], start=False, stop=True),
                                r=['Vt', ptk], w=[pko], inc=False)
                            S.op('pe', lambda e, psO=psO, lo=lo, PT=PT: e.matmul(
                                psO[lo:lo + 64, 128:256], ones[:, 0:64], PT[:, 0:128], start=True, stop=False),
                                r=['cst', ptk], w=[pko], inc=False)
                            S.op('pe', lambda e, psO=psO, lo=lo, PT=PT: e.matmul(
                                psO[lo:lo + 64, 128:256], ones[:, 0:64], PT[:, 128:256], start=False, stop=True),
                                r=['cst', ptk], w=[pko])
                        dN = yT[:, 4 + pr, qs:qs + r_ * 127 + 1:r_]
                        dD = accD[:, pr, qs:qs + r_ * 127 + 1:r_]
                        ykey = f"yT{4 + pr}"; dkey = f"accD{pr}"
                        if 'A' in os.environ.get('ATT_SKIP', ''):
                            pass
                        elif g == 0:
                            S.op('dve', lambda e, dN=dN, psO=psO: e.tensor_copy(out=dN, in_=psO[:, 0:128]), r=[pko], w=[ykey])
                            S.op('dve', lambda e, dD=dD, psO=psO: e.tensor_copy(out=dD, in_=psO[:, 128:256]), r=[pko], w=[dkey])
                        else:
                            S.op('dve', lambda e, dN=dN, psO=psO: e.tensor_tensor(out=dN, in0=psO[:, 0:128], in1=dN, op=ALU.add),
                                 r=[pko, ykey], w=[ykey])
                            S.op('dve', lambda e, dD=dD, psO=psO: e.tensor_tensor(out=dD, in0=psO[:, 128:256], in1=dD, op=ALU.add),
                                 r=[pko, dkey], w=[dkey])
                        blk_i += 1
        for pr in range(2):
            S.op('dve', lambda e, pr=pr: e.reciprocal(out=accD[:, pr, :], in_=accD[:, pr, :]), r=[f"accD{pr}"], w=[f"accD{pr}"])
            S.op('dve', lambda e, pr=pr: e.tensor_tensor(out=yT[:, 4 + pr, :], in0=yT[:, 4 + pr, :], in1=accD[:, pr, :], op=ALU.mult),
                 r=[f"accD{pr}", f"yT{4 + pr}"], w=[f"yT{4 + pr}"])
        S.barrier()


    if stage in (12, 121):
        G.close()
        return nc

    with ExitStack() as ph:
        def sb(name, shape, dt=F32):
            return ph.enter_context(nc.sbuf_tensor(uname(name), list(shape), dt))
        lbt = sb("lbt", [128, 8]); lb = sb("lb", [128, 4]); oml = sb("oml", [128, 4])
        S.dma('sp', lambda e: e.dma_start(out=lbt[:], in_=lbT_d.ap()), w=['lbt'])
        S.op('dve', lambda e: e.tensor_tensor(out=lb[:], in0=lbt[:, 0:4], in1=lbt[:, 4:8], op=ALU.subtract), r=['lbt'], w=['lb'])
        S.op('act', lambda e: e.activation(out=lb[:], in_=lb[:], func=AF.Sigmoid), r=['lb'], w=['lb'])
        S.op('dve', lambda e: e.tensor_scalar(out=oml[:], in0=lb[:], scalar1=-1.0, scalar2=1.0, op0=ALU.mult, op1=ALU.add), r=['lb'], w=['oml'])
        normw = sb("normw", [128, 512])
        S.dma('sp', lambda e: e.dma_start(out=normw[:], in_=normw_d.ap().partition_broadcast(128)), w=['normw'])
        wh = sb("wh", [128, 4, KC, 128])
        hsb = [sb(f"hsb{i}", [128, KC, 512]) for i in range(2)]
        state = [sb(f"st{i}", [128, 128]) for i in range(2)]
        sig = sb("sig", [128, 512]); fT = sb("fT", [128, 512]); lfT = sb("lfT", [128, 512]); kkT = sb("kkT", [128, 512])
        bT = sb("bT", [128, 512]); e1 = sb("e1", [128, 512]); kdT = sb("kdT", [128, 512])
        ebT = sb("ebT", [128, 512]); enb = sb("enb", [128, 512]); qe = sb("qe", [128, 512]); ke = sb("ke", [128, 512])
        i_sb = sb("i_sb", [128, 4, 128]); kd_sb = sb("kd_sb", [128, 4, 128]); g_sb = sb("g_sb", [128, 4, 128])
        sT = [sb(f"sT{i}", [128, 64]) for i in range(2)]
        ssq = sb("ssq", [128, 1]); rs = sb("rs", [128, 1]); junk = sb("junk", [128, 128])
        t1 = sb("t1", [128, 128]); yv = sb("yv", [128, 128]); eblast = sb("eblast", [128, 8])
        slab_i = 0
        for h in range(4):
            for j in range(4):
                S.dma('sp', lambda e, j=j, h=h: e.dma_start(out=wh[:, j, :, :], in_=w_inv[:, :, j * 512 + h * 128:j * 512 + (h + 1) * 128]),
                      w=['wh'])
            S.op('pool', lambda e: e.memset(state[0][:], 0.0), w=['st0'])
            ci = 0
            for si in range(8):
                own = si >= 4
                c0 = si * 512
                hs = hsb[slab_i % 2]; hkey = f"hsb{slab_i % 2}"; slab_i += 1
                S.dma('sp', lambda e, hs=hs, c0=c0: e.dma_start(out=hs[:], in_=HTv[:, :, c0:c0 + 512]), r=[f"HT{si}"], w=[hkey])
                for k in range(KC):
                    S.op('pe', lambda e, k=k, hs=hs: e.matmul(ps[0][:, :], wh[:, 1, k, :], hs[:, k, :], start=(k == 0), stop=(k == KC - 1)),
                         r=['wh', hkey], w=['ps0'], inc=(k == KC - 1))
                S.op('act', lambda e: e.activation(out=sig[:], in_=ps[0][:, :], func=AF.Sigmoid), r=['ps0'], w=['sig'])
                S.op('dve', lambda e, h=h: e.tensor_scalar(out=fT[:], in0=sig[:], scalar1=oml[:, h:h + 1], scalar2=lb[:, h:h + 1],
                                                          op0=ALU.mult, op1=ALU.add), r=['sig', 'oml', 'lb'], w=['fT'])
                S.op('act', lambda e: e.activation(out=lfT[:], in_=fT[:], func=AF.Ln), r=['fT'], w=['lfT'])
                S.op('pool', lambda e: e.tensor_scalar(out=kkT[:], in0=fT[:], scalar1=-1.0, scalar2=1.0, op0=ALU.mult, op1=ALU.add),
                     r=['fT'], w=['kkT'])
                for c in range(8):
                    S.op('dve', lambda e, c=c: e.tensor_tensor_scan(out=bT[:, c * 64:(c + 1) * 64], data0=ones[:, 0:64],
                                                                   data1=lfT[:, c * 64:(c + 1) * 64], initial=0.0,
                                                                   op0=ALU.mult, op1=ALU.add), r=['lfT', 'cst'], w=['bT'])
                blast = bT[:].rearrange("p (c s) -> p c s", s=64)[:, :, 63]
                S.op('act', lambda e: e.activation(out=eblast[:], in_=blast, func=AF.Exp), r=['bT'], w=['eblast'])
                for c in range(8):
                    S.op('act', lambda e, c=c: e.activation(out=e1[:, c * 64:(c + 1) * 64], in_=bT[:, c * 64:(c + 1) * 64], func=AF.Exp,
                                                           bias=bT[:, c * 64 + 63:c * 64 + 64], scale=-1.0), r=['bT'], w=['e1'])
                S.op('pool', lambda e: e.tensor_tensor(out=kdT[:], in0=e1[:], in1=kkT[:], op=ALU.mult), r=['e1', 'kkT'], w=['kdT'])
                for j in range(4):
                    for k in range(KC):
                        S.op('pe', lambda e, k=k, j=j, hs=hs: e.matmul(ps[2][:, j * 128:(j + 1) * 128], hs[:, k, j * 128:(j + 1) * 128],
                                                                    wh[:, 2, k, :], start=(k == 0), stop=(k == KC - 1)),
                             r=['wh', hkey], w=['ps2'], inc=(k == KC - 1))
                if own:
                    S.op('act', lambda e: e.activation(out=i_sb[:].rearrange("p a b -> p (a b)"), in_=ps[2][:, :], func=AF.Copy),
                         r=['ps2'], w=['i_sb'])
                else:
                    S.op('dve', lambda e: e.tensor_scalar(out=i_sb[:].rearrange("p a b -> p (a b)"), in0=ps[2][:, :], scalar1=flag[:, 0:1],
                                                        scalar2=None, op0=ALU.mult), r=['ps2', 'flag'], w=['i_sb'])
                for j in range(4):
                    S.op('pe', lambda e, j=j: e.transpose(out=ps[4][:, j * 128:(j + 1) * 128], in_=kdT[:, j * 128:(j + 1) * 128], identity=ident),
                         r=['kdT', 'cst'], w=['ps4'], inc=(j == 3))
                S.op('dve', lambda e: e.tensor_copy(out=kd_sb[:].rearrange("p a b -> p (a b)"), in_=ps[4][:, :]), r=['ps4'], w=['kd_sb'])
                if own:
                    for k in range(KC):
                        S.op('pe', lambda e, k=k, hs=hs: e.matmul(ps[1][:, :], wh[:, 0, k, :], hs[:, k, :], start=(k == 0), stop=(k == KC - 1)),
                             r=['wh', hkey], w=['ps1'], inc=(k == KC - 1))
                    S.op('act', lambda e: e.activation(out=ebT[:], in_=bT[:], func=AF.Exp), r=['bT'], w=['ebT'])
                    S.op('dve', lambda e: e.tensor_tensor(out=qe[:], in0=ps[1][:, :], in1=ebT[:], op=ALU.mult), r=['ps1', 'ebT'], w=['qe'])
                    S.op('act', lambda e: e.activation(out=enb[:], in_=bT[:], func=AF.Exp, scale=-1.0), r=['bT'], w=['enb'])
                    S.op('pool', lambda e: e.tensor_tensor(out=ke[:], in0=kkT[:], in1=enb[:], op=ALU.mult), r=['kkT', 'enb'], w=['ke'])
                    for j in range(4):
                        for k in range(KC):
                            S.op('pe', lambda e, k=k, j=j, hs=hs: e.matmul(ps[3][:, j * 128:(j + 1) * 128], hs[:, k, j * 128:(j + 1) * 128],
                                                                        wh[:, 3, k, :], start=(k == 0), stop=(k == KC - 1)),
                                 r=['wh', hkey], w=['ps3'], inc=(k == KC - 1))
                    S.op('act', lambda e: e.activation(out=g_sb[:].rearrange("p a b -> p (a b)"), in_=ps[3][:, :], func=AF.Silu),
                         r=['ps3'], w=['g_sb'])
                for c in range(8):
                    j = c // 2; b0 = (c % 2) * 64
                    cs = slice(c * 64, (c + 1) * 64)
                    cur = state[ci % 2]; nxt = state[(ci + 1) % 2]
                    ck = f"st{ci % 2}"; nk = f"st{(ci + 1) % 2}"
                    ci += 1
                    o6 = ps[6][:, (j % 2) * 128:(j % 2) * 128 + 128]; o6k = "ps6"
                    if own:
                        sTt = sT[c % 2]; sTk = f"sT{c % 2}"
                        S.op('pe', lambda e, b0=b0, cs=cs: e.matmul(ps[5][b0:b0 + 64, 0:64], ke[:, cs], qe[:, cs], start=True, stop=True),
                             r=['ke', 'qe'], w=['ps5'])
                        S.op('dve', lambda e, b0=b0, sTt=sTt: e.tensor_tensor(out=sTt[b0:b0 + 64, :], in0=ps[5][b0:b0 + 64, 0:64],
                                                                            in1=cmask[b0:b0 + 64, :], op=ALU.mult),
                             r=['ps5', 'cst'], w=[sTk])
                        S.op('pe', lambda e, b0=b0, cs=cs, cur=cur, o6=o6: e.matmul(o6[b0:b0 + 64, :], qe[:, cs], cur[:, :], start=True, stop=False),
                             r=['qe', ck], w=[o6k], inc=False)
                        S.op('pe', lambda e, b0=b0, sTt=sTt, j=j, o6=o6: e.matmul(o6[b0:b0 + 64, :], sTt[b0:b0 + 64, :], i_sb[b0:b0 + 64, j, :],
                                                                            start=False, stop=True), r=[sTk, 'i_sb'], w=[o6k])
                    u7 = ps[7][:, (c % 2) * 128:(c % 2) * 128 + 128]; u7k = "ps7"
                    S.op('pe', lambda e, b0=b0, j=j, u7=u7: e.matmul(u7, kd_sb[b0:b0 + 64, j, :], i_sb[b0:b0 + 64, j, :], start=True, stop=True),
                         r=['kd_sb', 'i_sb'], w=[u7k])
                    S.op('dve', lambda e, cur=cur, nxt=nxt, c=c, u7=u7: e.scalar_tensor_tensor(
                        out=nxt[:], in0=cur[:], scalar=eblast[:, c:c + 1], in1=u7, op0=ALU.mult, op1=ALU.add),
                        r=[ck, 'eblast', u7k], w=[nk])
                    if own and c % 2 == 1:
                        S.op('act', lambda e, o6=o6: e.activation(out=junk[:], in_=o6, func=AF.Square, accum_out=ssq[:]), r=[o6k], w=['junk', 'ssq'])
                        S.op('act', lambda e: e.activation(out=rs[:], in_=ssq[:], func=AF.Sqrt, bias=epsb[:, 1:2], scale=1.0 / 128.0),
                             r=['ssq', 'epsb'], w=['rs'])
                        S.op('dve', lambda e: e.reciprocal(out=rs[:], in_=rs[:]), r=['rs'], w=['rs'])
                        S.op('dve', lambda e, o6=o6, h=h: e.scalar_tensor_tensor(out=t1[:], in0=o6, scalar=rs[:, 0:1],
                                                                               in1=normw[:, h * 128:(h + 1) * 128], op0=ALU.mult, op1=ALU.mult),
                             r=[o6k, 'rs', 'normw'], w=['t1'])
                        S.op('pool', lambda e, j=j: e.tensor_tensor(out=yv[:], in0=t1[:], in1=g_sb[:, j, :], op=ALU.mult), r=['t1', 'g_sb'], w=['yv'])
                        S.op('pe', lambda e: e.transpose(out=ps[5][:, 256:384], in_=yv[:], identity=ident), r=['yv', 'cst'], w=['ps5'])
                        tok0 = (si - 4) * 512 + j * 128
                        S.op('act', lambda e, h=h, tok0=tok0: e.activation(out=yT[:, h, tok0:tok0 + 128], in_=ps[5][:, 256:384], func=AF.Copy),
                             r=['ps5'], w=[f"yT{h}"])
        S.barrier()

    if stage == 13:
        G.close()
        return nc

    x1 = gsb("x1", [128, NT, D])
    with ExitStack() as ph:
        def sb(name, shape, dt=F32):
            return ph.enter_context(nc.sbuf_tensor(uname(name), list(shape), dt))
        wo = sb("wo", [128, 6, D])
        S.dma('sp', lambda e: e.dma_start(out=wo[:], in_=w_out.ap().rearrange("(k p) n -> p k n", p=128)), w=['wo'])
        lg = sb("lg", [128, D]); lbb = sb("lbb", [128, D])
        S.dma('sp', lambda e: e.dma_start(out=lg[:], in_=ln1g.ap().partition_broadcast(128)), w=['lg'])
        S.dma('sp', lambda e: e.dma_start(out=lbb[:], in_=ln1b.ap().partition_broadcast(128)), w=['lbb'])
        xb = [sb(f"xb{i}", [128, D]) for i in range(2)]
        tb = [sb(f"tb{i}", [128, D]) for i in range(2)]
        st = sb("st", [128, 2, 6]); mv = sb("mv", [128, 2]); rstd = sb("rstd", [128, 1])
        for tt in range(NT):
            xt = xb[tt % 2]; xk = f"xb{tt % 2}"
            S.dma('sp', lambda e, tt=tt, xt=xt: e.dma_start(out=xt[:], in_=xo.ap()[tt * 128:(tt + 1) * 128, :]), w=[xk])
            t = tb[tt % 2]; tk = f"tb{tt % 2}"
            for hf in range(2):
                pi = (tt % 2) * 2 + hf
                for k in range(6):
                    S.op('pe', lambda e, k=k, hf=hf, tt=tt, pi=pi: e.matmul(ps[pi][:, :], yT[:, k, tt * 128:(tt + 1) * 128],
                                                                     wo[:, k, hf * 512:(hf + 1) * 512], start=(k == 0), stop=(k == 5)),
                         r=['wo'] + [f"yT{k}"], w=[PK[pi]], inc=(k == 5))
                S.op('dve', lambda e, hf=hf, pi=pi, t=t: e.tensor_tensor(out=t[:, hf * 512:(hf + 1) * 512], in0=ps[pi][:, :],
                                                                   in1=g1b[:, hf * 512:(hf + 1) * 512], op=ALU.mult),
                     r=[PK[pi], 'g1b'], w=[tk])
            S.op('dve', lambda e, xt=xt, t=t: e.scalar_tensor_tensor(out=t[:], in0=xt[:], scalar=ALPHA, in1=t[:], op0=ALU.mult, op1=ALU.add),
                 r=[xk, tk], w=[tk])
            layer_norm_stats('dve', t, tk, st, mv, rstd, 'p4')
            S.op('dve', lambda e, tt=tt, t=t: e.tensor_scalar(out=x1[:, tt, :], in0=t[:], scalar1=mv[:, 0:1], scalar2=rstd[:, 0:1],
                                                        op0=ALU.subtract, op1=ALU.mult), r=[tk, 'p4mv', 'p4rs'], w=[f"x1_{tt}"])
            S.op('pool', lambda e, tt=tt: e.tensor_tensor(out=x1[:, tt, :], in0=x1[:, tt, :], in1=lg[:], op=ALU.mult), r=[f"x1_{tt}", 'lg'], w=[f"x1_{tt}"])
            S.op('pool', lambda e, tt=tt: e.tensor_tensor(out=x1[:, tt, :], in0=x1[:, tt, :], in1=lbb[:], op=ALU.add), r=[f"x1_{tt}", 'lbb'], w=[f"x1_{tt}"])
            if stage == 2:
                S.dma('sp', lambda e, tt=tt: e.dma_start(out=out_d.ap()[tt * 128:(tt + 1) * 128, :], in_=x1[:, tt, :]), r=[f"x1_{tt}"], w=['out'])
        S.barrier()

    if stage == 2:
        G.close()
        return nc


    dest_all = gsb("dest_all", [128, NT, 8], I32)
    wk_all = gsb("wk_all", [128, NT, 8])
    H2v = H2.ap()
    bc_reg = nc.gpsimd.to_reg(NROW - 1)
    with ExitStack() as ph:
        def sb(name, shape, dt=F32):
            return ph.enter_context(nc.sbuf_tensor(uname(name), list(shape), dt))
        wr = sb("wr", [128, KC, NE])
        S.dma('sp', lambda e: e.dma_start(out=wr[:], in_=w_router.ap().rearrange("(k p) n -> p k n", p=128)), w=['wr'])
        rbb = sb("rbb", [128, NE])
        S.dma('sp', lambda e: e.dma_start(out=rbb[:], in_=rbias.ap().partition_broadcast(128)), w=['rbb'])
        cum = sb("cum", [128, NE])
        S.op('pool', lambda e: e.memset(cum[:], 0.0), w=['cum'])
        h2b = [sb(f"h2_{i}", [128, D]) for i in range(2)]
        xn2 = sb("xn2", [128, D]); h2T = sb("h2T", [128, KC, 128])
        st = sb("st", [128, 2, 6]); mv = sb("mv", [128, 2]); rstd = sb("rstd", [128, 1])
        scores = sb("scores", [128, NE]); biased = sb("biased", [128, NE]); m8 = sb("m8", [128, 8, 8])
        gs = sb("gs", [128, 8]); g8 = sb("g8", [128, 8]); gmask = sb("gmask", [128, 8]); negoff = sb("negoff", [128, 8])
        masked = sb("masked", [128, NE]); t8 = sb("t8", [128, 8]); sel = sb("sel", [128, NE]); ssel = sb("ssel", [128, NE])
        ssum = sb("ssum", [128, 1]); Wc = sb("Wc", [128, NE]); sel2 = sb("sel2", [128, NE]); Vp = sb("Vp", [128, NE])
        d8 = sb("d8", [128, 8]); junk2 = sb("junk2", [128, NE]); dtr = sb("dtr", [128, 8])
        for tt in range(NT):
            xk = f"x1_{tt}"
            layer_norm_stats('dve', x1[:, tt, :], xk, st, mv, rstd, 'p5')
            S.op('dve', lambda e, tt=tt: e.tensor_scalar(out=xn2[:], in0=x1[:, tt, :], scalar1=mv[:, 0:1], scalar2=rstd[:, 0:1],
                                                        op0=ALU.subtract, op1=ALU.mult), r=[xk, 'p5mv', 'p5rs'], w=['xn2'])
            h2 = h2b[tt % 2]; hk = f"h2_{tt % 2}"
            S.op('pool', lambda e, h2=h2: e.tensor_tensor(out=h2[:], in0=xn2[:], in1=sc2b[:], op=ALU.mult), r=['xn2', 'sc2b'], w=[hk])
            S.op('pool', lambda e, h2=h2: e.tensor_tensor(out=h2[:], in0=h2[:], in1=sh2b[:], op=ALU.add), r=[hk, 'sh2b'], w=[hk])
            S.dma('sp', lambda e, tt=tt, h2=h2: e.dma_start(out=H2v[tt * 128:(tt + 1) * 128, :], in_=h2[:]), r=[hk], w=['H2'])
            for hb_ in range(2):
                for q in range(4):
                    k = hb_ * 4 + q
                    S.op('pe', lambda e, k=k, q=q, hb_=hb_, h2=h2: e.transpose(out=ps[hb_][:, q * 128:(q + 1) * 128],
                                                                        in_=h2[:, k * 128:(k + 1) * 128], identity=ident),
                         r=[hk, 'cst'], w=[PK[hb_]], inc=(q == 3))
            S.op('act', lambda e: e.activation(out=h2T[:, 0:4, :].rearrange("p a b -> p (a b)"), in_=ps[0][:, :], func=AF.Copy), r=['ps0'], w=['h2Ta'])
            S.op('dve', lambda e: e.tensor_copy(out=h2T[:, 4:8, :].rearrange("p a b -> p (a b)"), in_=ps[1][:, :]), r=['ps1'], w=['h2Tb'])
            for k in range(KC):
                S.op('pe', lambda e, k=k: e.matmul(ps[2][:, 0:NE], h2T[:, k, :], wr[:, k, :], start=(k == 0), stop=(k == KC - 1)),
                     r=['h2Ta', 'h2Tb', 'wr'], w=['ps2'], inc=(k == KC - 1))
            S.op('act', lambda e: e.activation(out=scores[:], in_=ps[2][:, 0:NE], func=AF.Sigmoid), r=['ps2'], w=['scores'])
            S.op('dve', lambda e: e.tensor_tensor(out=biased[:], in0=scores[:], in1=rbb[:], op=ALU.add), r=['scores', 'rbb'], w=['biased'])
            for gi in range(8):
                S.op('dve', lambda e, gi=gi: e.max(out=m8[:, gi, :], in_=biased[:, gi * 32:(gi + 1) * 32]), r=['biased'], w=['m8'])
            S.op('dve', lambda e: e.tensor_tensor(out=gs[:], in0=m8[:, :, 0], in1=m8[:, :, 1], op=ALU.add), r=['m8'], w=['gs'])
            S.op('dve', lambda e: e.max(out=g8[:], in_=gs[:]), r=['gs'], w=['g8'])
            S.op('dve', lambda e: e.tensor_scalar(out=gmask[:], in0=gs[:], scalar1=g8[:, 3:4], scalar2=None, op0=ALU.is_ge),
                 r=['gs', 'g8'], w=['gmask'])
            S.op('dve', lambda e: e.tensor_scalar(out=negoff[:], in0=gmask[:], scalar1=10.0, scalar2=-10.0, op0=ALU.mult, op1=ALU.add),
                 r=['gmask'], w=['negoff'])
            for gi in range(8):
                S.op('dve', lambda e, gi=gi: e.tensor_scalar(out=masked[:, gi * 32:(gi + 1) * 32], in0=biased[:, gi * 32:(gi + 1) * 32],
                                                            scalar1=gmask[:, gi:gi + 1], scalar2=negoff[:, gi:gi + 1],
                                                            op0=ALU.mult, op1=ALU.add), r=['biased', 'gmask', 'negoff'], w=['masked'])
            S.op('dve', lambda e: e.max(out=t8[:], in_=masked[:]), r=['masked'], w=['t8'])
            S.op('dve', lambda e: e.tensor_scalar(out=sel[:], in0=masked[:], scalar1=t8[:, 7:8], scalar2=None, op0=ALU.is_ge),
                 r=['masked', 't8'], w=['sel'])
            S.op('dve', lambda e: e.scalar_tensor_tensor(out=ssel[:], in0=scores[:], scalar=1.0, in1=sel[:], op0=ALU.mult, op1=ALU.mult,
                                                        accum_out=ssum[:]), r=['scores', 'sel'], w=['ssel', 'ssum'])
            S.op('dve', lambda e: e.reciprocal(out=ssum[:], in_=ssum[:]), r=['ssum'], w=['ssum'])
            S.op('dve', lambda e: e.tensor_scalar(out=Wc[:], in0=ssel[:], scalar1=ssum[:, 0:1], scalar2=2.5, op0=ALU.mult, op1=ALU.mult),
                 r=['ssel', 'ssum'], w=['Wc'])
            S.op('pe', lambda e: e.matmul(ps[3][:, 0:NE], Umat, sel[:], start=True, stop=False), r=['sel', 'cst'], w=['ps3'], inc=False)
            S.op('pe', lambda e: e.matmul(ps[3][:, 0:NE], ones, cum[:], start=False, stop=True), r=['cum', 'cst'], w=['ps3'])
            S.op('dve', lambda e: e.tensor_tensor(out=Vp[:], in0=ps[3][:, 0:NE], in1=iota1, op=ALU.add), r=['ps3', 'cst'], w=['Vp'])
            S.op('dve', lambda e: e.tensor_tensor(out=sel2[:], in0=Vp[:], in1=limv, op=ALU.is_lt), r=['Vp', 'cst'], w=['sel2'])
            S.op('dve', lambda e: e.tensor_tensor(out=sel2[:], in0=sel2[:], in1=sel[:], op=ALU.mult), r=['sel2', 'sel'], w=['sel2'])
            S.op('dve', lambda e: e.tensor_tensor(out=Vp[:], in0=Vp[:], in1=sel2[:], op=ALU.mult), r=['Vp', 'sel2'], w=['Vp'])
            S.op('dve', lambda e: e.tensor_tensor(out=Wc[:], in0=Wc[:], in1=sel2[:], op=ALU.mult), r=['Wc', 'sel2'], w=['Wc'])
            S.op('pool', lambda e: e.tensor_tensor(out=cum[:], in0=cum[:], in1=sel[:], op=ALU.add), r=['cum', 'sel'], w=['cum'])
            S.op('dve', lambda e: e.max(out=d8[:], in_=Vp[:]), r=['Vp'], w=['d8'])
            S.op('dve', lambda e: e.tensor_scalar(out=dtr[:], in0=d8[:], scalar1=0.0, scalar2=pidx, op0=ALU.is_equal, op1=ALU.mult),
                 r=['d8', 'cst'], w=['dtr'])
            S.op('dve', lambda e, tt=tt: e.scalar_tensor_tensor(out=dest_all[:, tt, :], in0=d8[:], scalar=-1.0, in1=dtr[:],
                                                                op0=ALU.add, op1=ALU.add), r=['d8', 'dtr'], w=[f"dest{tt}"])
            for k in range(8):
                S.op('dve', lambda e, k=k, tt=tt: e.scalar_tensor_tensor(out=junk2[:], in0=Vp[:], scalar=d8[:, k:k + 1], in1=Wc[:],
                                                                        op0=ALU.is_equal, op1=ALU.mult,
                                                                        accum_out=wk_all[:, tt, k:k + 1]),
                     r=['Vp', 'd8', 'Wc'], w=['junk2', f"wk{tt}"])
            for half in range(0 if 'D' in os.environ.get('MOE_SKIP', '') else 2):
                S.idma_batch([
                    (lambda e, k=k, tt=tt, h2=h2: e.indirect_dma_start(
                        out=Xg.ap(), out_offset=bass.IndirectOffsetOnAxis(ap=dest_all[:, tt, k:k + 1], axis=0),
                        in_=h2[:], in_offset=None, bounds_check=bc_reg, oob_is_err=False))
                    for k in range(half * 4, half * 4 + 4)], r=[hk, f"dest{tt}"], w=['Xg'])
        S.barrier()

    if stage == 25:
        dbgt = gsb("dbgt", [128, NT, 16])
        S.op('dve', lambda e: e.tensor_copy(out=dbgt[:, :, 0:8], in_=dest_all[:]), w=['dbgt'])
        S.op('dve', lambda e: e.tensor_copy(out=dbgt[:, :, 8:16], in_=wk_all[:]), w=['dbgt'])
        S.dma('sp', lambda e: e.dma_start(out=out_d.ap()[0:128, 0:256], in_=dbgt[:].rearrange("p a b -> p (a b)")), r=['dbgt'], w=['out'])
        S.barrier()
        G.close()
        return nc

    with ExitStack() as ph:
        def sb(name, shape, dt=F32):
            return ph.enter_context(nc.sbuf_tensor(uname(name), list(shape), dt))
        wgu = [sb(f"wgu{i}", [128, KC, 512], F32R) for i in range(2)]
        wd = [sb(f"wd{i}", [128, 2, D], F32R) for i in range(2)]
        yTf = yT[:].rearrange("p a b -> p (a b)")
        wguS = [yTf[:, i * 6144:i * 6144 + 4096].rearrange("p (k f) -> p k f", k=KC) for i in range(2)]
        wdS = [yTf[:, i * 6144 + 4096:(i + 1) * 6144].rearrange("p (k n) -> p k n", k=2) for i in range(2)]
        xg = [sb("xg0", [128, D]), sh2b]
        yg = [sb("yg0", [128, D]), g1b]
        xgT = [sb(f"xgT{i}", [128, KC, 128], F32R) for i in range(2)]
        sg = [sb(f"sg{i}", [128, 256]) for i in range(2)]
        hb = [sb(f"hb{i}", [128, 256]) for i in range(2)]
        hbT = [sb(f"hbT{i}", [128, 2, 128], F32R) for i in range(2)]
        NEX = int(os.environ.get('MOE_NEX', NE))
        S.op('pool', lambda e: e.memset(yg[0][:], 0.0), w=['yg0'])
        S.dma('sp', lambda e: e.dma_start(out=Yg.ap()[TRASH:TRASH + 128, :], in_=yg[0][:]), r=['yg0'], w=['Yg'])

        def load_w(e_):
            i = e_ % 2
            S.dma('sp', lambda e: e.dma_start(out=wguS[i][:, :, 0:256], in_=w_eg.ap()[e_].rearrange("(k p) f -> p k f", p=128)), w=[f"wguS{i}"])
            S.dma('sp', lambda e: e.dma_start(out=wguS[i][:, :, 256:512], in_=w_eu.ap()[e_].rearrange("(k p) f -> p k f", p=128)), w=[f"wguS{i}"])
            S.dma('sp', lambda e: e.dma_start(out=wdS[i], in_=w_ed.ap()[e_].rearrange("(k p) n -> p k n", p=128)), w=[f"wdS{i}"])
            S.op('pool', lambda e: e.tensor_copy(out=wgu[i][:], in_=wguS[i]), r=[f"wguS{i}"], w=[f"wgu{i}"])
            S.op('pool', lambda e: e.tensor_copy(out=wd[i][:], in_=wdS[i]), r=[f"wdS{i}"], w=[f"wd{i}"])

        def load_x(eb):
            xi = eb % 2
            S.dma('sp', lambda e: e.dma_start(out=xg[xi][:], in_=Xg.ap()[eb * 128:(eb + 1) * 128, :]), w=[f"xg{xi}"])
        NBLK = CAP // 128
        NB = NEX * NBLK - (1 if NEX == NE else 0)
        load_w(0)
        load_x(0)
        for eb in range(NB):
            e_ = eb // NBLK; blk = eb % NBLK
            if blk == 0 and e_ + 1 < NEX:
                load_w(e_ + 1)
            if eb + 1 < NB:
                load_x(eb + 1)
            i = e_ % 2
            xi = eb % 2
            tb = (0, 1) if xi == 0 else (6, 7)
            gub = 2 + xi
            for hb_ in range(2):
                for q in range(4):
                    k = hb_ * 4 + q
                    S.op('pe', lambda e, k=k, q=q, hb_=hb_, xi=xi, tb=tb: e.transpose(out=ps[tb[hb_]][:, q * 128:(q + 1) * 128],
                                                                               in_=xg[xi][:, k * 128:(k + 1) * 128], identity=ident),
                         r=[f"xg{xi}", 'cst'], w=[PK[tb[hb_]]], inc=(q == 3))
            S.op('act', lambda e, xi=xi, tb=tb: e.activation(out=xgT[xi][:, 0:4, :].rearrange("p a b -> p (a b)"), in_=ps[tb[0]][:, :], func=AF.Copy),
                 r=[PK[tb[0]]], w=[f"xgTa{xi}"])
            S.op('dve', lambda e, xi=xi, tb=tb: e.tensor_copy(out=xgT[xi][:, 4:8, :].rearrange("p a b -> p (a b)"), in_=ps[tb[1]][:, :]),
                 r=[PK[tb[1]]], w=[f"xgTb{xi}"])
            for k in range(KC):
                S.op('pe', lambda e, k=k, i=i, xi=xi, gub=gub: e.matmul(ps[gub][:, :], xgT[xi][:, k, :], wgu[i][:, k, :],
                                                                 start=(k == 0), stop=(k == KC - 1)),
                     r=[f"xgTa{xi}", f"xgTb{xi}", f"wgu{i}"], w=[PK[gub]], inc=(k == KC - 1))
            S.op('act', lambda e, xi=xi, gub=gub: e.activation(out=sg[xi][:], in_=ps[gub][:, 0:256], func=AF.Silu), r=[PK[gub]], w=[f"sg{xi}"])
            S.op('dve', lambda e, xi=xi, gub=gub: e.tensor_tensor(out=hb[xi][:], in0=ps[gub][:, 256:512], in1=sg[xi][:], op=ALU.mult),
                 r=[PK[gub], f"sg{xi}"], w=[f"hb{xi}"])
            for fc in range(2):
                S.op('pe', lambda e, fc=fc, xi=xi, gub=gub: e.transpose(out=ps[gub][:, fc * 128:(fc + 1) * 128],
                                                                 in_=hb[xi][:, fc * 128:(fc + 1) * 128], identity=ident),
                     r=[f"hb{xi}", 'cst'], w=[PK[gub]], inc=(fc == 1))
            S.op('dve', lambda e, xi=xi, gub=gub: e.tensor_copy(out=hbT[xi][:].rearrange("p a b -> p (a b)"), in_=ps[gub][:, 0:256]),
                 r=[PK[gub]], w=[f"hbT{xi}"])
            for hf in range(2):
                for fc in range(2):
                    S.op('pe', lambda e, fc=fc, hf=hf, i=i, xi=xi: e.matmul(ps[4 + hf][:, :], hbT[xi][:, fc, :],
                                                                     wd[i][:, fc, hf * 512:(hf + 1) * 512],
                                                                     start=(fc == 0), stop=(fc == 1)),
                         r=[f"hbT{xi}", f"wd{i}"], w=[PK[4 + hf]], inc=(fc == 1))
            S.op('act', lambda e, xi=xi: e.activation(out=yg[xi][:, 0:512], in_=ps[4][:, :], func=AF.Copy), r=['ps4'], w=[f"yg{xi}"])
            S.op('dve', lambda e, xi=xi: e.tensor_copy(out=yg[xi][:, 512:1024], in_=ps[5][:, :]), r=['ps5'], w=[f"yg{xi}"])
            S.dma('act', lambda e, xi=xi, eb=eb: e.dma_start(out=Yg.ap()[eb * 128:(eb + 1) * 128, :], in_=yg[xi][:]), r=[f"yg{xi}"], w=['Yg'])
        S.barrier()

    with ExitStack() as ph:
        def sb(name, shape, dt=F32):
            return ph.enter_context(nc.sbuf_tensor(uname(name), list(shape), dt))
        wsgu = sb("wsgu", [128, KC, 512]); wsd = sb("wsd", [128, 2, D])
        S.dma('sp', lambda e: e.dma_start(out=wsgu[:, :, 0:256], in_=w_sg.ap().rearrange("(k p) f -> p k f", p=128)), w=['wsgu'])
        S.dma('sp', lambda e: e.dma_start(out=wsgu[:, :, 256:512], in_=w_su.ap().rearrange("(k p) f -> p k f", p=128)), w=['wsgu'])
        S.dma('sp', lambda e: e.dma_start(out=wsd[:], in_=w_sd.ap().rearrange("(k p) n -> p k n", p=128)), w=['wsd'])
        lg2 = sb("lg2", [128, D]); lb2 = sb("lb2", [128, D])
        S.dma('sp', lambda e: e.dma_start(out=lg2[:], in_=ln2g.ap().partition_broadcast(128)), w=['lg2'])
        S.dma('sp', lambda e: e.dma_start(out=lb2[:], in_=ln2b.ap().partition_broadcast(128)), w=['lb2'])
        h2r = sb("h2r", [128, D]); h2T = sb("h2T", [128, KC, 128]); sg = sb("sg", [128, 256]); hs = sb("hs", [128, 256])
        hsT = sb("hsT", [128, 2, 128]); acc = sb("acc", [128, D]); ot = sb("ot", [128, D])
        gb = [sb(f"gb{i}", [128, D]) for i in range(4)]
        st = sb("st", [128, 2, 6]); mv = sb("mv", [128, 2]); rstd = sb("rstd", [128, 1])
        for i in range(4):
            S.op('pool', lambda e, i=i: e.memset(gb[i][:], 0.0), w=[f"gb{i}"])
        for tt in range(NT):
            S.dma('sp', lambda e, tt=tt: e.dma_start(out=h2r[:], in_=H2v[tt * 128:(tt + 1) * 128, :]), w=['h2r'])
            for hb_ in range(2):
                for q in range(4):
                    k = hb_ * 4 + q
                    S.op('pe', lambda e, k=k, q=q, hb_=hb_: e.transpose(out=ps[hb_][:, q * 128:(q + 1) * 128],
                                                                 in_=h2r[:, k * 128:(k + 1) * 128], identity=ident),
                         r=['h2r', 'cst'], w=[PK[hb_]], inc=(q == 3))
            S.op('act', lambda e: e.activation(out=h2T[:, 0:4, :].rearrange("p a b -> p (a b)"), in_=ps[0][:, :], func=AF.Copy), r=['ps0'], w=['h2Ta'])
            S.op('dve', lambda e: e.tensor_copy(out=h2T[:, 4:8, :].rearrange("p a b -> p (a b)"), in_=ps[1][:, :]), r=['ps1'], w=['h2Tb'])
            for k in range(KC):
                S.op('pe', lambda e, k=k: e.matmul(ps[2][:, :], h2T[:, k, :], wsgu[:, k, :], start=(k == 0), stop=(k == KC - 1)),
                     r=['h2Ta', 'h2Tb', 'wsgu'], w=['ps2'], inc=(k == KC - 1))
            S.op('act', lambda e: e.activation(out=sg[:], in_=ps[2][:, 0:256], func=AF.Silu), r=['ps2'], w=['sg'])
            S.op('dve', lambda e: e.tensor_tensor(out=hs[:], in0=ps[2][:, 256:512], in1=sg[:], op=ALU.mult), r=['ps2', 'sg'], w=['hs'])
            for fc in range(2):
                S.op('pe', lambda e, fc=fc: e.transpose(out=ps[3][:, fc * 128:(fc + 1) * 128], in_=hs[:, fc * 128:(fc + 1) * 128], identity=ident),
                     r=['hs', 'cst'], w=['ps3'], inc=(fc == 1))
            S.op('dve', lambda e: e.tensor_copy(out=hsT[:].rearrange("p a b -> p (a b)"), in_=ps[3][:, 0:256]), r=['ps3'], w=['hsT'])
            for hf in range(2):
                for fc in range(2):
                    S.op('pe', lambda e, fc=fc, hf=hf: e.matmul(ps[4 + hf][:, :], hsT[:, fc, :], wsd[:, fc, hf * 512:(hf + 1) * 512],
                                                          start=(fc == 0), stop=(fc == 1)), r=['hsT', 'wsd'], w=[PK[4 + hf]], inc=(fc == 1))
            S.op('dve', lambda e: e.tensor_copy(out=acc[:, 0:512], in_=ps[4][:, :]), r=['ps4'], w=['acc'])
            S.op('dve', lambda e: e.tensor_copy(out=acc[:, 512:1024], in_=ps[5][:, :]), r=['ps5'], w=['acc'])
            for half in range(2):
                S.idma_batch([
                    (lambda e, k=k, tt=tt, j=j: e.indirect_dma_start(
                        out=gb[j][:], out_offset=None, in_=Yg.ap(),
                        in_offset=bass.IndirectOffsetOnAxis(ap=dest_all[:, tt, k:k + 1], axis=0),
                        bounds_check=bc_reg, oob_is_err=False))
                    for j, k in enumerate(range(half * 4, half * 4 + 4))], r=['Yg', f"dest{tt}"], w=[f"gb{j}" for j in range(4)])
                for j, k in enumerate(range(half * 4, half * 4 + 4)):
                    S.op('dve', lambda e, j=j, k=k, tt=tt: e.scalar_tensor_tensor(out=acc[:], in0=gb[j][:], scalar=wk_all[:, tt, k:k + 1],
                                                                              in1=acc[:], op0=ALU.mult, op1=ALU.add),
                         r=[f"gb{j}", f"wk{tt}", 'acc'], w=['acc'])
            S.op('pool', lambda e: e.tensor_tensor(out=acc[:], in0=acc[:], in1=g2b[:], op=ALU.mult), r=['acc', 'g2b'], w=['acc'])
            S.op('dve', lambda e, tt=tt: e.scalar_tensor_tensor(out=acc[:], in0=x1[:, tt, :], scalar=ALPHA, in1=acc[:], op0=ALU.mult, op1=ALU.add),
                 r=[f"x1_{tt}", 'acc'], w=['acc'])
            layer_norm_stats('dve', acc, 'acc', st, mv, rstd, 'p7')
            S.op('dve', lambda e: e.tensor_scalar(out=ot[:], in0=acc[:], scalar1=mv[:, 0:1], scalar2=rstd[:, 0:1],
                                                op0=ALU.subtract, op1=ALU.mult), r=['acc', 'p7mv', 'p7rs'], w=['ot'])
            S.op('pool', lambda e: e.tensor_tensor(out=ot[:], in0=ot[:], in1=lg2[:], op=ALU.mult), r=['ot', 'lg2'], w=['ot'])
            S.op('pool', lambda e: e.tensor_tensor(out=ot[:], in0=ot[:], in1=lb2[:], op=ALU.add), r=['ot', 'lb2'], w=['ot'])
            S.dma('sp', lambda e, tt=tt: e.dma_start(out=out_d.ap()[tt * 128:(tt + 1) * 128, :], in_=ot[:]), r=['ot'], w=['out'])
        S.barrier()
    G.close()
    return nc


def make_consts():
    c = np.zeros((128, 1280), np.float32)
    c[:, 0:128] = np.eye(128, dtype=np.float32)
    tp = np.arange(128)[:, None]; tf = np.arange(128)[None, :]
    c[:, 128:256] = (tp < tf).astype(np.float32)
    c[:, 256:384] = 1.0
    sp_ = (np.arange(128) % 64)[:, None]; t_ = np.arange(64)[None, :]
    c[:, 384:448] = (sp_ <= t_).astype(np.float32)
    c[:, 512:768] = (np.arange(256) * CAP + 1)[None, :].astype(np.float32)
    c[:, 448] = (TRASH + 1 + np.arange(128)).astype(np.float32)
    lim = np.arange(256) * CAP + 1 + CAP
    lim[255] -= 128
    c[:, 1024:1280] = lim[None, :].astype(np.float32)
    ki = np.arange(128)[:, None]; qi = np.arange(128)[None, :]
    c[:, 768:896] = np.where(qi <= ki, 0.0, NEG)
    c[:, 896:1024] = np.where(ki <= qi, 0.0, NEG)
    return c


def make_bias_idx():
    ki = np.arange(128)[:, None]; qi = np.arange(128)[None, :]
    idx = np.zeros((3, 2, 128, 128), np.int64)
    for g, r in enumerate(DIL):
        idx[g, 0] = t5_bucket(np.clip(128 + qi - ki, 0, 10 ** 6) * r)
        idx[g, 1] = t5_bucket(np.clip(qi - ki, 0, 10 ** 6) * r)
    return idx


def prep_inputs(inp):
    f = lambda a: np.ascontiguousarray(np.asarray(a, dtype=np.float32))
    x = f(inp['x']); c = f(inp['c'])
    cst = make_consts()
    idx = make_bias_idx()
    rb = f(inp['rel_bias'])
    biasT = np.zeros((128, 12, 2, 128), np.float32)
    for h in range(12):
        g = h // 4
        for pc in range(2):
            biasT[:, h, pc, :] = rb[idx[g, pc], h]
    biasT = biasT.reshape(128, -1)
    hlb = f(inp['hg_lower_bound'])
    lbT = np.concatenate([hlb[0].reshape(4, 128).T, hlb[1].reshape(4, 128).T], axis=1)
    shared = {
        'w_ada': f(inp['w_ada'][0]), 'b_adaT': f(inp['b_ada'][0]).reshape(48, 128).T.copy(), 'b_ada': f(inp['b_ada']),
        'w_in': f(inp['w_in'][0]), 'lbT': np.ascontiguousarray(lbT), 'normw': f(inp['hg_norm_w']),
        'biasT': biasT, 'w_out': f(inp['w_out'][0]), 'ln1g': f(inp['ln1_g']), 'ln1b': f(inp['ln1_b']),
        'w_router': f(inp['w_router'][0]), 'rbias': f(inp['router_bias']),
        'w_eg': f(inp['w_e_gate'][0]), 'w_eu': f(inp['w_e_up'][0]), 'w_ed': f(inp['w_e_down'][0]),
        'w_sg': f(inp['w_sh_gate'][0]), 'w_su': f(inp['w_sh_up'][0]), 'w_sd': f(inp['w_sh_down'][0]),
        'ln2g': f(inp['ln2_g']), 'ln2b': f(inp['ln2_b']), 'cst': cst,
    }
    maps = []
    for core in range(8):
        b, half = core // 2, core % 2
        m = dict(shared)
        m['xo'] = x[b, half * S_OWN:(half + 1) * S_OWN]
        m['xp'] = x[b, 0:S_OWN] if half == 1 else np.zeros((S_OWN, D), np.float32)
        fl = np.zeros((128, 2), np.float32)
        fl[:, 0] = float(half); fl[:, 1] = 0.0 if half == 1 else NEG
        m['flag'] = fl
        m['cT'] = np.ascontiguousarray(c[b].reshape(8, 128).T)
        maps.append(m)
    return maps


_NC_CACHE = {}


def kernel(**inputs):
    maps = prep_inputs(inputs)
    nc = build(3)
    names = set(a.memorylocations[0].name for a in nc.allocations
                if isinstance(a, mybir.MemoryLocationSet) and a.kind == "ExternalInput")
    maps = [{k: v for k, v in m.items() if k in names} for m in maps]
    res = run_bass_kernel_spmd(nc, maps, core_ids=list(range(8)))
    out = np.zeros((4, 2 * S_OWN, D), np.float32)
    for core in range(8):
        b, half = core // 2, core % 2
        out[b, half * S_OWN:(half + 1) * S_OWN] = res.results[core]['out']
    return out
```
